# Optimizing a Trainium2 kernel written in Bass

```python
import jax, jax.numpy as jnp
from jax import lax
import numpy as np

D_MODEL = 4096
BATCH = 2
SEQ = 4096
DEPTH = 2

CHUNK = 64
N_EVEN = (DEPTH + 1) // 2
N_ODD = DEPTH // 2

POOL_WIDTH = D_MODEL // 2
POOL_WINDOWS = (2, 4, 8, 16)
N_POOL_GROUPS = len(POOL_WINDOWS)
POOL_GROUP_DIM = POOL_WIDTH // N_POOL_GROUPS
HEAD_DIM = 128
FOX_HEADS = (D_MODEL // 2) // HEAD_DIM
FOX_WIDTH = FOX_HEADS * HEAD_DIM
Q_BLOCK = 128
MIX_WIDTH = POOL_WIDTH + FOX_WIDTH
EVEN_IN_COLS = POOL_WIDTH + 3 * FOX_WIDTH + FOX_HEADS
LRU_WIDTH = D_MODEL
LRU_BLOCKS = 16
LRU_BLOCK_DIM = LRU_WIDTH // LRU_BLOCKS
CONV_WIDTH = 4
LRU_C = 8.0
N_GROUPS = 4
EXPERTS_PER_GROUP = 8
N_EXPERTS = N_GROUPS * EXPERTS_PER_GROUP
EXPERT_FF = 512
TOP_K = 2
DN_ALPHA = (2.0 * DEPTH) ** 0.25
DN_BETA = (8.0 * DEPTH) ** -0.25
LN_EPS = 1e-5

kernel_name = "hybrid_pool_fox_rglru_hmoe_deepnorm"


def layer_norm(x, g, b):
    xf = x.astype(jnp.float32)
    mu = jnp.mean(xf, axis=-1, keepdims=True)
    var = jnp.mean(jnp.square(xf - mu), axis=-1, keepdims=True)
    return ((xf - mu) * lax.rsqrt(var + LN_EPS) * g + b).astype(x.dtype)


def pool_mixer(u, w_pool, pool_scale):
    B, S, _ = u.shape
    uf = u.astype(jnp.float32).reshape(B, S, N_POOL_GROUPS, POOL_GROUP_DIM)
    cs = jnp.cumsum(uf, axis=1)
    t = jnp.arange(S)
    outs = []
    for g, w in enumerate(POOL_WINDOWS):
        cs_g = cs[:, :, g]
        lag = jnp.pad(cs_g, ((0, 0), (w, 0), (0, 0)))[:, :S]
        cnt = jnp.minimum(t + 1, w).astype(jnp.float32)[None, :, None]
        outs.append((cs_g - lag) / cnt - uf[:, :, g])
    pooled = jnp.stack(outs, axis=2).astype(u.dtype)
    mixed = jnp.einsum('bsgc,gcd->bsgd', pooled, w_pool)
    return mixed.reshape(B, S, POOL_WIDTH) * pool_scale


def forgetting_attention(q, k, v, f_logit, b_f):
    B, S, H, Dh = q.shape
    log_f = jax.nn.log_sigmoid(f_logit.astype(jnp.float32) + b_f.astype(jnp.float32))
    c = jnp.cumsum(log_f, axis=1).transpose(0, 2, 1)
    qh = q.transpose(0, 2, 1, 3)
    kh = k.transpose(0, 2, 1, 3)
    vh = v.transpose(0, 2, 1, 3)
    nb = S // Q_BLOCK
    q_blocks = qh.reshape(B, H, nb, Q_BLOCK, Dh).transpose(2, 0, 1, 3, 4)
    c_blocks = c.reshape(B, H, nb, Q_BLOCK).transpose(2, 0, 1, 3)
    k_pos = jnp.arange(S)
    scale = HEAD_DIM ** -0.5

    def block(args):
        qb, cq, idx = args
        s = jnp.einsum('bhqd,bhkd->bhqk', qb, kh).astype(jnp.float32) * scale
        s = s + cq[..., None] - c[:, :, None, :]
        q_pos = idx * Q_BLOCK + jnp.arange(Q_BLOCK)
        s = jnp.where(k_pos[None, :] <= q_pos[:, None], s, -jnp.inf)
        p = jax.nn.softmax(s, axis=-1)
        return jnp.einsum('bhqk,bhkd->bhqd', p.astype(vh.dtype), vh)

    out = lax.map(block, (q_blocks, c_blocks, jnp.arange(nb)))
    return out.transpose(1, 0, 3, 2, 4).reshape(B, S, H * Dh)


def even_mixer(x, w_in, w_pool, pool_scale, b_f, w_out):
    B, S, _ = x.shape
    proj = jnp.einsum('bsd,dc->bsc', x, w_in)
    u_pool, q, k, v, f = jnp.split(
        proj, [POOL_WIDTH, POOL_WIDTH + FOX_WIDTH, POOL_WIDTH + 2 * FOX_WIDTH,
               POOL_WIDTH + 3 * FOX_WIDTH], axis=-1)
    a_out = pool_mixer(u_pool, w_pool, pool_scale)
    hs = (B, S, FOX_HEADS, HEAD_DIM)
    b_out = forgetting_attention(q.reshape(hs), k.reshape(hs), v.reshape(hs), f, b_f)
    mixed = jnp.concatenate([a_out, b_out], axis=-1)
    return jnp.einsum('bsc,cd->bsd', mixed, w_out)


def rglru_mixer(x, w_in, conv_w, conv_b, w_a, b_a, w_x, b_x, lam, w_out):
    B, S, _ = x.shape
    proj = jnp.einsum('bsd,dc->bsc', x, w_in)
    gate, xb = jnp.split(proj, [LRU_WIDTH], axis=-1)
    gate = jax.nn.gelu(gate)
    xp = jnp.pad(xb, ((0, 0), (CONV_WIDTH - 1, 0), (0, 0)))
    xc = conv_b + sum(xp[:, i:i + S] * conv_w[i] for i in range(CONV_WIDTH))
    xh = xc.reshape(B, S, LRU_BLOCKS, LRU_BLOCK_DIM)
    r = jax.nn.sigmoid(jnp.einsum('bshi,hij->bshj', xh, w_a) + b_a)
    ig = jax.nn.sigmoid(jnp.einsum('bshi,hij->bshj', xh, w_x) + b_x)
    log_a_base = -jax.nn.softplus(-lam.astype(jnp.float32)).reshape(LRU_BLOCKS, LRU_BLOCK_DIM)
    log_a = LRU_C * r.astype(jnp.float32) * log_a_base
    a = jnp.exp(log_a)
    mult = jnp.sqrt(-jnp.expm1(2.0 * log_a))
    bterm = mult * ig.astype(jnp.float32) * xh.astype(jnp.float32)

    def combine(lhs, rhs):
        a1, b1 = lhs
        a2, b2 = rhs
        return a1 * a2, a2 * b1 + b2

    _, h = lax.associative_scan(combine, (a, bterm), axis=1)
    y = h.reshape(B, S, LRU_WIDTH).astype(x.dtype) * gate
    return jnp.einsum('bsc,cd->bsd', y, w_out)


def hierarchical_moe(x, w_group, b_group, w_expert, b_expert, w1, w3, w2):
    B, S, D = x.shape
    xt = x.reshape(B * S, D)
    g_logits = (xt @ w_group + b_group).astype(jnp.float32)
    g_prob = jax.nn.softmax(g_logits, axis=-1)
    g_idx = jnp.argmax(g_logits, axis=-1)
    g_w = jnp.take_along_axis(g_prob, g_idx[:, None], axis=-1)
    e_logits = (xt @ w_expert + b_expert).astype(jnp.float32).reshape(-1, N_GROUPS, EXPERTS_PER_GROUP)
    e_sel = jnp.take_along_axis(e_logits, g_idx[:, None, None], axis=1)[:, 0]
    top_v, top_i = lax.top_k(e_sel, TOP_K)
    weights = jax.nn.softmax(top_v, axis=-1) * g_w
    expert_id = g_idx[:, None] * EXPERTS_PER_GROUP + top_i
    gates = jnp.sum(jax.nn.one_hot(expert_id, N_EXPERTS, dtype=jnp.float32) * weights[..., None], axis=1)
    h = jax.nn.silu(jnp.einsum('td,edf->tef', xt, w1)) * jnp.einsum('td,edf->tef', xt, w3)
    h = h * gates[..., None].astype(h.dtype)
    y = jnp.einsum('tef,efd->td', h, w2)
    return y.reshape(B, S, D)


def setup_inputs(seed: int = 0) -> dict:
    key = jax.random.key(seed)
    ks = jax.random.split(key, 32)

    def nrm(k, shape, scale):
        return jax.random.normal(k, shape, jnp.float32) * scale

    a0 = jax.random.uniform(ks[13], (N_ODD, LRU_WIDTH), jnp.float32, 0.9, 0.999)
    p = a0 ** (1.0 / LRU_C)
    lam = jnp.log(p) - jnp.log1p(-p)
    return {
        "x": nrm(ks[0], (BATCH, SEQ, D_MODEL), 1.0),
        "even_w_in": nrm(ks[1], (N_EVEN, D_MODEL, EVEN_IN_COLS), D_MODEL ** -0.5),
        "even_w_pool": nrm(ks[2], (N_EVEN, N_POOL_GROUPS, POOL_GROUP_DIM, POOL_GROUP_DIM), POOL_GROUP_DIM ** -0.5),
        "even_pool_scale": 1.0 + nrm(ks[3], (N_EVEN, POOL_WIDTH), 0.1),
        "even_b_f": jax.random.uniform(ks[4], (N_EVEN, FOX_HEADS), jnp.float32, 1.0, 4.0),
        "even_w_out": nrm(ks[5], (N_EVEN, MIX_WIDTH, D_MODEL), DN_BETA * MIX_WIDTH ** -0.5),
        "odd_w_in": nrm(ks[6], (N_ODD, D_MODEL, 2 * LRU_WIDTH), D_MODEL ** -0.5),
        "odd_conv_w": nrm(ks[7], (N_ODD, CONV_WIDTH, LRU_WIDTH), CONV_WIDTH ** -0.5),
        "odd_conv_b": nrm(ks[8], (N_ODD, LRU_WIDTH), 0.02),
        "odd_w_a": nrm(ks[9], (N_ODD, LRU_BLOCKS, LRU_BLOCK_DIM, LRU_BLOCK_DIM), LRU_BLOCK_DIM ** -0.5),
        "odd_b_a": nrm(ks[10], (N_ODD, LRU_BLOCKS, LRU_BLOCK_DIM), 0.1),
        "odd_w_x": nrm(ks[11], (N_ODD, LRU_BLOCKS, LRU_BLOCK_DIM, LRU_BLOCK_DIM), LRU_BLOCK_DIM ** -0.5),
        "odd_b_x": nrm(ks[12], (N_ODD, LRU_BLOCKS, LRU_BLOCK_DIM), 0.1),
        "odd_lambda": lam,
        "odd_w_out": nrm(ks[14], (N_ODD, LRU_WIDTH, D_MODEL), DN_BETA * LRU_WIDTH ** -0.5),
        "moe_w_group": nrm(ks[15], (DEPTH, D_MODEL, N_GROUPS), D_MODEL ** -0.5),
        "moe_b_group": nrm(ks[16], (DEPTH, N_GROUPS), 0.01),
        "moe_w_expert": nrm(ks[17], (DEPTH, D_MODEL, N_EXPERTS), D_MODEL ** -0.5),
        "moe_b_expert": nrm(ks[18], (DEPTH, N_EXPERTS), 0.01),
        "moe_w1": nrm(ks[19], (DEPTH, N_EXPERTS, D_MODEL, EXPERT_FF), D_MODEL ** -0.5),
        "moe_w3": nrm(ks[20], (DEPTH, N_EXPERTS, D_MODEL, EXPERT_FF), D_MODEL ** -0.5),
        "moe_w2": nrm(ks[21], (DEPTH, N_EXPERTS, EXPERT_FF, D_MODEL), DN_BETA * EXPERT_FF ** -0.5),
        "ln_g": 1.0 + nrm(ks[22], (DEPTH, 2, D_MODEL), 0.05),
        "ln_b": nrm(ks[23], (DEPTH, 2, D_MODEL), 0.02),
    }


def reference(x, even_w_in, even_w_pool, even_pool_scale, even_b_f, even_w_out,
              odd_w_in, odd_conv_w, odd_conv_b, odd_w_a, odd_b_a, odd_w_x, odd_b_x,
              odd_lambda, odd_w_out, moe_w_group, moe_b_group, moe_w_expert, moe_b_expert,
              moe_w1, moe_w3, moe_w2, ln_g, ln_b):
    for layer in range(DEPTH):
        i = layer // 2
        if layer % 2 == 0:
            mix = even_mixer(x, even_w_in[i], even_w_pool[i], even_pool_scale[i],
                             even_b_f[i], even_w_out[i])
        else:
            mix = rglru_mixer(x, odd_w_in[i], odd_conv_w[i], odd_conv_b[i], odd_w_a[i],
                              odd_b_a[i], odd_w_x[i], odd_b_x[i], odd_lambda[i], odd_w_out[i])
        x = layer_norm(DN_ALPHA * x + mix, ln_g[layer, 0], ln_b[layer, 0])
        ffn = hierarchical_moe(x, moe_w_group[layer], moe_b_group[layer], moe_w_expert[layer],
                               moe_b_expert[layer], moe_w1[layer], moe_w3[layer], moe_w2[layer])
        x = layer_norm(DN_ALPHA * x + ffn, ln_g[layer, 1], ln_b[layer, 1])
    return x
```

```python
import contextlib
import numpy as np
import concourse.bass as bass
import concourse.mybir as mybir
from concourse.bass_utils import run_bass_kernel_spmd

F32 = mybir.dt.float32
BF16 = mybir.dt.bfloat16
AF = mybir.ActivationFunctionType
ALU = mybir.AluOpType
AX = mybir.AxisListType

D = 4096
DC = 32
SEQ = 4096
BATCH = 2
TP = 512
NE = 32
FF = 512
ALPHA = 4.0 ** 0.25
EPS = 1e-5
BIG = 1.0e4
NSLOT = 8
SEM_LIMIT = 30000
HEAD_DIM = 128
LRU_C = 8.0


class Buf:
    __slots__ = ("name", "w", "r")

    def __init__(self, name=""):
        self.name = name
        self.w = None
        self.r = {}


class Op:
    __slots__ = ("lane", "fn", "deps", "idx", "signal", "sigval")

    def __init__(self, lane, fn):
        self.lane = lane
        self.fn = fn
        self.deps = {}
        self.idx = 0
        self.signal = False
        self.sigval = None


class Lane:
    def __init__(self, name, issuer, inc, is_dma):
        self.name = name
        self.issuer = issuer
        self.inc = inc
        self.is_dma = is_dma
        self.ops = []


class Prog:
    def __init__(self, nc):
        self.nc = nc
        self.ops = []
        self.lanes = {}
        for n in ("tensor", "vector", "scalar", "gpsimd"):
            self.lanes[n] = Lane(n, n, 1, False)
        self.eng = {"tensor": nc.tensor, "vector": nc.vector, "scalar": nc.scalar,
                    "gpsimd": nc.gpsimd, "sync": nc.sync}

    def dma_lane(self, issuer, cls):
        key = "dma_%s_%s" % (issuer, cls)
        if key not in self.lanes:
            self.lanes[key] = Lane(key, issuer, 16, True)
        return key

    def op(self, lane, fn, reads=(), writes=()):
        ln = self.lanes[lane]
        o = Op(ln, fn)
        o.idx = len(ln.ops) + 1
        ln.ops.append(o)
        deps = o.deps

        def add(d, war=False):
            if d is None:
                return
            if d.lane is ln and not ln.is_dma:
                if ln.name == "tensor" or war:
                    return
            cur = deps.get(d.lane.name)
            if cur is None or cur.idx < d.idx:
                deps[d.lane.name] = d

        for b in reads:
            add(b.w)
        for b in writes:
            add(b.w)
            for rd in b.r.values():
                add(rd, war=True)
        for b in reads:
            b.r[ln.name] = o
        for b in writes:
            b.w = o
            b.r = {}
        self.ops.append(o)
        return o

    def dma(self, issuer, cls, out, in_, reads=(), writes=()):
        lane = self.dma_lane(issuer, cls)
        eng = self.eng[issuer]
        return self.op(lane, lambda: eng.dma_start(out=out, in_=in_), reads, writes)

    def emit(self, es):
        nc = self.nc
        waited = {}
        plan = []
        for o in self.ops:
            wd = waited.setdefault(o.lane.issuer, {})
            waits = []
            for lname, d in o.deps.items():
                if wd.get(lname, 0) >= d.idx:
                    continue
                waits.append(d)
                wd[lname] = d.idx
            plan.append(waits)
        for waits in plan:
            for d in waits:
                d.signal = True
        final_lanes = [l.name for l in self.lanes.values() if l.is_dma and l.ops]
        for lname in final_lanes:
            self.lanes[lname].ops[-1].signal = True
        sems = {}

        def get_sem(lname, epoch):
            k = (lname, epoch)
            if k not in sems:
                sems[k] = es.enter_context(nc.semaphore("s_%s_%d" % (lname, epoch)))
            return sems[k]

        for ln in self.lanes.values():
            cnt = 0
            epoch = 0
            for o in ln.ops:
                if ln.is_dma or o.signal:
                    o.signal = True
                    if cnt + ln.inc > SEM_LIMIT:
                        epoch += 1
                        cnt = 0
                    cnt += ln.inc
                    o.sigval = (epoch, cnt)
        nwaits = 0
        for o, waits in zip(self.ops, plan):
            eng = self.eng[o.lane.issuer]
            for d in waits:
                ep, v = d.sigval
                eng.wait_ge(get_sem(d.lane.name, ep), v)
                nwaits += 1
            ins = o.fn()
            if o.signal:
                ep, v = o.sigval
                ins.then_inc(get_sem(o.lane.name, ep), o.lane.inc)
        for lname in final_lanes:
            ep, v = self.lanes[lname].ops[-1].sigval
            nc.sync.wait_ge(get_sem(lname, ep), v)
        return dict(n_ops=len(self.ops), n_waits=nwaits, n_sems=len(sems))


class Ring:
    def __init__(self, P, tiles, bufs):
        self.P = P
        self.tiles = tiles
        self.bufs = bufs
        self.n = len(tiles)
        self.queue = []
        self.loaded = 0
        self.used = 0
        self.released = 0

    def push(self, loader):
        self.queue.append(loader)

    def _pump(self):
        while self.loaded < len(self.queue) and self.loaded < self.released + self.n:
            s = self.loaded % self.n
            self.queue[self.loaded](self.tiles[s], self.bufs[s])
            self.loaded += 1

    def take(self):
        if self.used >= self.loaded:
            self._pump()
        assert self.used < self.loaded
        s = self.used % self.n
        self.used += 1
        return self.tiles[s], self.bufs[s]

    def release(self, k=1):
        self.released += k
        assert self.released <= self.used
        self._pump()


def build_tail(T=1024, n_exp=NE, with_proj=True):
    nc = bass.Bass("TRN2", target_bir_lowering=False)
    dt = nc.dram_tensor
    xres = dt("xres", [D, T], F32, kind="ExternalInput").ap()
    if with_proj:
        mix = dt("mix", [D, T], F32, kind="ExternalInput").ap()
        wout = dt("wout", [D, D], F32, kind="ExternalInput").ap()
    wr = dt("wr", [D, 36], F32, kind="ExternalInput").ap()
    br = dt("br", [128, 36], F32, kind="ExternalInput").ap()
    w1 = dt("w1", [n_exp, D, FF], F32, kind="ExternalInput").ap()
    w3 = dt("w3", [n_exp, D, FF], F32, kind="ExternalInput").ap()
    w2 = dt("w2", [n_exp, FF, D], F32, kind="ExternalInput").ap()
    lnp = dt("lnp", [128, 4, DC], F32, kind="ExternalInput").ap()
    ident_d = dt("ident", [128, 128], F32, kind="ExternalInput").ap()
    sel_d = dt("sel", [32, NE * 128], F32, kind="ExternalInput").ap()
    xout = dt("xout", [D, T], F32, kind="ExternalOutput").ap()

    es = contextlib.ExitStack()
    with es:
        def sb(name, shape, dtype):
            return es.enter_context(nc.sbuf_tensor(name, shape, dtype))

        ACC = sb("ACC", [128, DC, TP], F32)
        XB = sb("XB", [128, DC, TP], BF16)
        RINGT = [sb("ring%d" % i, [128, 4096], BF16) for i in range(NSLOT)]
        HT = [sb("HT%d" % i, [128, 4, TP], BF16) for i in range(2)]
        S = [sb("S%d" % i, [128, TP], F32) for i in range(4)]
        TT = [sb("TT%d" % i, [128, TP], F32) for i in range(2)]
        GBC = [sb("GBC%d" % i, [128, TP], F32) for i in range(2)]
        WR = sb("WR", [128, DC, 36], F32)
        BR = sb("BR", [128, 36], F32)
        IDENT = sb("IDENT", [128, 128], F32)
        ONES = sb("ONES", [128, 128], F32)
        LNP = sb("LNP", [128, 4, DC], F32)
        GT = sb("GT", [32, TP], F32)
        MEAN = sb("MEAN", [128, TP], F32)
        RSTD = sb("RSTD", [128, TP], F32)
        SQ = [sb("SQ%d" % i, [128, TP], F32) for i in range(2)]
        OUT = [sb("OUT%d" % i, [128, TP], F32) for i in range(2)]
        LG = sb("LG", [128, 36], F32)
        R = {n: sb("r_" + n, [128, 32], F32) for n in ("em", "em2", "m1", "m2", "g1", "gates", "gexp")}
        C = {n: sb("c_" + n, [128, 4], F32) for n in
             ("gmax", "ngmax", "gsum", "gw", "goh", "pen", "v1", "v2", "d", "ed", "den", "w1", "w2")}
        PS = [es.enter_context(nc.psum_tensor("ps%d" % i, [128, 512], F32)) for i in range(8)]
        es.enter_context(nc.Block())

        P = Prog(nc)
        bACC = [Buf("acc%d" % c) for c in range(DC)]
        bXB = Buf("xb")
        bHT = [Buf(), Buf()]
        bS = [Buf(), Buf(), Buf(), Buf()]
        bTT = [Buf(), Buf()]
        bGBC = [Buf(), Buf()]
        bPS = [Buf("ps%d" % i) for i in range(8)]
        bC = Buf("consts")
        bR = Buf("router")
        bGT = Buf("gt")
        bMEAN = Buf("mean")
        bRSTD = Buf("rstd")
        bSQ = [Buf(), Buf()]
        bOUT = [Buf(), Buf()]
        ring = Ring(P, RINGT, [Buf("ring%d" % i) for i in range(NSLOT)])
        PH, PY, PGB, PMISC = (0, 1, 2, 3), (4, 5), 6, 7
        V, A, PE, G = nc.vector, nc.scalar, nc.tensor, nc.gpsimd

        P.dma("sync", "c", WR[:], wr.rearrange("(c p) n -> p c n", p=128), writes=[bC])
        P.dma("sync", "c", BR[:], br, writes=[bC])
        P.dma("sync", "c", IDENT[:], ident_d, writes=[bC])
        P.dma("sync", "c", LNP[:], lnp, writes=[bC])
        P.op("vector", lambda: V.memset(ONES[:], 1.0 / D), writes=[bC])

        def ld_cols(src2d, col0):
            def f(tile, buf):
                srcv = src2d.rearrange("(c p) f -> p c f", p=128)
                dst = tile[:].rearrange("p (c f) -> p c f", f=128)
                for q in range(2):
                    P.dma("gpsimd", "w", dst[:, q * 16:(q + 1) * 16, :],
                          srcv[:, q * 16:(q + 1) * 16, col0:col0 + 128], writes=[buf])
            return f

        def ld_krange(src2d, kp):
            def f(tile, buf):
                srcv = src2d.rearrange("(c p) f -> p c f", p=128)
                dst = tile[:].rearrange("p (c f) -> p c f", f=512)
                P.dma("gpsimd", "w", dst, srcv[:, kp * 8:(kp + 1) * 8, :], writes=[buf])
            return f

        def ld_w2(src2d, g):
            def f(tile, buf):
                srcv = src2d.rearrange("(j p) d -> p j d", p=128)
                dst = tile[:].rearrange("p (j d) -> p j d", d=1024)
                P.dma("gpsimd", "w", dst, srcv[:, :, g * 1024:(g + 1) * 1024], writes=[buf])
            return f

        npass = T // TP
        for ps_i in range(npass):
            if with_proj:
                for dc in range(DC):
                    ring.push(ld_cols(wout, dc * 128))
            for e in range(n_exp):
                for kp in range(4):
                    ring.push(ld_krange(w1[e], kp))
                for kp in range(4):
                    ring.push(ld_krange(w3[e], kp))
                for g in range(4):
                    ring.push(ld_w2(w2[e], g))

        def layer_norm(kg, kb, store_t0=None):
            pm, pq = PY[0], PY[1]
            for c in range(DC):
                P.op("tensor", (lambda c=c: PE.matmul(PS[pm][:], lhsT=ONES[:], rhs=ACC[:, c, :],
                                                       start=(c == 0), stop=(c == DC - 1))),
                     reads=[bC, bACC[c]], writes=[bPS[pm]])
            for c in range(DC):
                qb = c % 2
                P.op("scalar", (lambda c=c, qb=qb: A.activation(out=SQ[qb][:], in_=ACC[:, c, :], func=AF.Square)),
                     reads=[bACC[c]], writes=[bSQ[qb]])
                P.op("tensor", (lambda c=c, qb=qb: PE.matmul(PS[pq][:], lhsT=ONES[:], rhs=SQ[qb][:],
                                                              start=(c == 0), stop=(c == DC - 1))),
                     reads=[bC, bSQ[qb]], writes=[bPS[pq]])
            P.op("vector", lambda: V.tensor_copy(out=MEAN[:], in_=PS[pm][:]), reads=[bPS[pm]], writes=[bMEAN])
            P.op("vector", lambda: V.tensor_tensor(out=RSTD[:], in0=MEAN[:], in1=MEAN[:], op=ALU.mult),
                 reads=[bMEAN], writes=[bRSTD])
            P.op("vector", lambda: V.tensor_tensor(out=RSTD[:], in0=PS[pq][:], in1=RSTD[:], op=ALU.subtract),
                 reads=[bPS[pq], bRSTD], writes=[bRSTD])
            P.op("vector", lambda: V.tensor_scalar(out=RSTD[:], in0=RSTD[:], scalar1=EPS, scalar2=None, op0=ALU.add),
                 reads=[bRSTD], writes=[bRSTD])
            P.op("scalar", lambda: A.activation(out=RSTD[:], in_=RSTD[:], func=AF.Sqrt), reads=[bRSTD], writes=[bRSTD])
            P.op("vector", lambda: V.reciprocal(out=RSTD[:], in_=RSTD[:]), reads=[bRSTD], writes=[bRSTD])
            ov = xout.rearrange("(c p) t -> p c t", p=128)
            for c in range(DC):
                P.op("vector", (lambda c=c: V.tensor_tensor(out=ACC[:, c, :], in0=ACC[:, c, :], in1=MEAN[:], op=ALU.subtract)),
                     reads=[bMEAN, bACC[c]], writes=[bACC[c]])
                P.op("vector", (lambda c=c: V.tensor_tensor(out=ACC[:, c, :], in0=ACC[:, c, :], in1=RSTD[:], op=ALU.mult)),
                     reads=[bRSTD, bACC[c]], writes=[bACC[c]])
                if store_t0 is None:
                    P.op("scalar", (lambda c=c: A.activation(out=ACC[:, c, :], in_=ACC[:, c, :], func=AF.Identity,
                                                              bias=LNP[:, kb, c:c + 1], scale=LNP[:, kg, c:c + 1])),
                         reads=[bACC[c], bC], writes=[bACC[c]])
                else:
                    ob = c % 2
                    P.op("scalar", (lambda c=c, ob=ob: A.activation(out=OUT[ob][:], in_=ACC[:, c, :], func=AF.Identity,
                                                                     bias=LNP[:, kb, c:c + 1], scale=LNP[:, kg, c:c + 1])),
                         reads=[bACC[c], bC], writes=[bOUT[ob]])
                    P.dma("sync", "out", ov[:, c, store_t0:store_t0 + TP], OUT[ob][:], reads=[bOUT[ob]])

        def scale_acc():
            for c in range(DC):
                P.op("gpsimd", (lambda c=c: G.tensor_scalar(out=ACC[:, c, :], in0=ACC[:, c, :], scalar1=ALPHA,
                                                             scalar2=0.0, op0=ALU.mult, op1=ALU.add)),
                     reads=[bACC[c]], writes=[bACC[c]])

        def router():
            rw = [bR]
            for s in range(TP // 128):
                ts = slice(s * 128, (s + 1) * 128)
                for c in range(DC):
                    P.op("tensor", (lambda c=c, ts=ts: PE.matmul(PS[PMISC][:, 0:36], lhsT=ACC[:, c, ts], rhs=WR[:, c, :],
                                                                   start=(c == 0), stop=(c == DC - 1))),
                         reads=[bACC[c], bC], writes=[bPS[PMISC]])

                def v(fn, extra_reads=()):
                    P.op("vector", fn, reads=rw + list(extra_reads), writes=rw)

                def a(fn):
                    P.op("scalar", fn, reads=rw, writes=rw)

                v(lambda: V.tensor_tensor(out=LG[:], in0=PS[PMISC][:, 0:36], in1=BR[:], op=ALU.add),
                  [bPS[PMISC], bC])
                v(lambda: V.reduce_max(out=C["gmax"][:, 0:1], in_=LG[:, 0:4], axis=AX.X))
                v(lambda: V.tensor_scalar(out=C["goh"][:, 0:4], in0=LG[:, 0:4], scalar1=C["gmax"][:, 0:1],
                                          scalar2=None, op0=ALU.is_ge))
                v(lambda: V.tensor_scalar(out=C["ngmax"][:, 0:1], in0=C["gmax"][:, 0:1], scalar1=-1.0,
                                          scalar2=None, op0=ALU.mult))
                a(lambda: A.activation(out=R["gexp"][:, 0:4], in_=LG[:, 0:4], func=AF.Exp,
                                       bias=C["ngmax"][:, 0:1], scale=1.0, accum_out=C["gsum"][:, 0:1]))
                v(lambda: V.reciprocal(out=C["gw"][:, 0:1], in_=C["gsum"][:, 0:1]))
                v(lambda: V.tensor_scalar(out=C["pen"][:, 0:4], in0=C["goh"][:, 0:4], scalar1=-1.0,
                                          scalar2=BIG, op0=ALU.add, op1=ALU.mult))
                for g in range(4):
                    v(lambda g=g: V.tensor_scalar(out=R["em"][:, g * 8:(g + 1) * 8],
                                                  in0=LG[:, 4 + g * 8:4 + (g + 1) * 8],
                                                  scalar1=C["pen"][:, g:g + 1], scalar2=None, op0=ALU.add))
                v(lambda: V.reduce_max(out=C["v1"][:, 0:1], in_=R["em"][:], axis=AX.X))
                v(lambda: V.tensor_scalar(out=R["m1"][:], in0=R["em"][:], scalar1=C["v1"][:, 0:1],
                                          scalar2=None, op0=ALU.is_ge))
                v(lambda: V.scalar_tensor_tensor(out=R["em2"][:], in0=R["m1"][:], scalar=-BIG, in1=R["em"][:],
                                                 op0=ALU.mult, op1=ALU.add))
                v(lambda: V.reduce_max(out=C["v2"][:, 0:1], in_=R["em2"][:], axis=AX.X))
                v(lambda: V.tensor_scalar(out=R["m2"][:], in0=R["em2"][:], scalar1=C["v2"][:, 0:1],
                                          scalar2=None, op0=ALU.is_ge))
                v(lambda: V.tensor_tensor(out=C["d"][:, 0:1], in0=C["v2"][:, 0:1], in1=C["v1"][:, 0:1],
                                          op=ALU.subtract))
                a(lambda: A.activation(out=C["ed"][:, 0:1], in_=C["d"][:, 0:1], func=AF.Exp))
                v(lambda: V.tensor_scalar(out=C["den"][:, 0:1], in0=C["ed"][:, 0:1], scalar1=1.0,
                                          scalar2=None, op0=ALU.add))
                v(lambda: V.reciprocal(out=C["w1"][:, 0:1], in_=C["den"][:, 0:1]))
                v(lambda: V.tensor_tensor(out=C["w2"][:, 0:1], in0=C["ed"][:, 0:1], in1=C["w1"][:, 0:1], op=ALU.mult))
                v(lambda: V.tensor_tensor(out=C["w1"][:, 0:1], in0=C["w1"][:, 0:1], in1=C["gw"][:, 0:1], op=ALU.mult))
                v(lambda: V.tensor_tensor(out=C["w2"][:, 0:1], in0=C["w2"][:, 0:1], in1=C["gw"][:, 0:1], op=ALU.mult))
                v(lambda: V.tensor_scalar(out=R["g1"][:], in0=R["m1"][:], scalar1=C["w1"][:, 0:1],
                                          scalar2=None, op0=ALU.mult))
                v(lambda: V.scalar_tensor_tensor(out=R["gates"][:], in0=R["m2"][:], scalar=C["w2"][:, 0:1],
                                                 in1=R["g1"][:], op0=ALU.mult, op1=ALU.add))
                P.op("tensor", lambda: PE.transpose(PS[PMISC][0:32, 128:256], R["gates"][:], IDENT[:]),
                     reads=[bR, bC], writes=[bPS[PMISC]])
                P.op("vector", (lambda ts=ts: V.tensor_copy(out=GT[:, ts], in_=PS[PMISC][0:32, 128:256])),
                     reads=[bPS[PMISC]], writes=[bGT])

        def cast_acc_to_xb():
            for c in range(DC):
                if c % 2 == 0:
                    P.op("vector", (lambda c=c: V.tensor_copy(out=XB[:, c, :], in_=ACC[:, c, :])),
                         reads=[bACC[c]], writes=[bXB])
                else:
                    P.op("scalar", (lambda c=c: A.copy(out=XB[:, c, :], in_=ACC[:, c, :])),
                         reads=[bACC[c]], writes=[bXB])

        def experts():
            for e in range(n_exp):
                hb = e % 2
                P.op("tensor", (lambda e=e: PE.matmul(PS[PGB][:], lhsT=IDENT[0:32, e:e + 1].to_broadcast([32, 128]), rhs=GT[:],
                                                       start=True, stop=True)),
                     reads=[bC, bGT], writes=[bPS[PGB]])
                P.op("scalar", (lambda hb=hb: A.copy(out=GBC[hb][:], in_=PS[PGB][:])),
                     reads=[bPS[PGB]], writes=[bGBC[hb]])
                for (kind, SRC) in ((0, None), (1, None)):
                    for kp in range(4):
                        tw, bw = ring.take()
                        Wv = tw[:].rearrange("p (c f) -> p c f", f=512)
                        for j in range(4):
                            for c in range(8):
                                P.op("tensor", (lambda c=c, j=j, kp=kp, Wv=Wv: PE.matmul(PS[PH[j]][:], lhsT=Wv[:, c, j * 128:(j + 1) * 128],
                                                                                         rhs=XB[:, kp * 8 + c, :],
                                                                                         start=(kp == 0 and c == 0), stop=(kp == 3 and c == 7))),
                                     reads=[bw, bXB], writes=[bPS[PH[j]]])
                        ring.release(1)
                    for j in range(4):
                        if kind == 0:
                            P.op("scalar", (lambda j=j: A.activation(out=S[j][:], in_=PS[PH[j]][:], func=AF.Silu)),
                                 reads=[bPS[PH[j]]], writes=[bS[j]])
                        else:
                            jb = j % 2
                            P.op("vector", (lambda j=j, jb=jb: V.tensor_tensor(out=TT[jb][:], in0=PS[PH[j]][:], in1=S[j][:], op=ALU.mult)),
                                 reads=[bPS[PH[j]], bS[j]], writes=[bTT[jb]])
                            P.op("vector", (lambda jb=jb, hb=hb, j=j: V.tensor_tensor(out=HT[hb][:, j, :], in0=TT[jb][:],
                                                                                       in1=GBC[hb][:], op=ALU.mult)),
                                 reads=[bTT[jb], bGBC[hb]], writes=[bHT[hb]])
                for g in range(4):
                    t2, b2 = ring.take()
                    W2v = t2[:].rearrange("p (j d) -> p j d", d=1024)
                    for dl in range(8):
                        dc = g * 8 + dl
                        py = PY[dc % 2]
                        for j in range(4):
                            P.op("tensor", (lambda j=j, dl=dl, W2v=W2v, py=py, hb=hb:
                                            PE.matmul(PS[py][:], lhsT=W2v[:, j, dl * 128:(dl + 1) * 128],
                                                      rhs=HT[hb][:, j, :], start=(j == 0), stop=(j == 3))),
                                 reads=[b2, bHT[hb]], writes=[bPS[py]])
                        P.op("vector", (lambda dc=dc, py=py: V.tensor_tensor(out=ACC[:, dc, :], in0=PS[py][:],
                                                                              in1=ACC[:, dc, :], op=ALU.add)),
                             reads=[bPS[py], bACC[dc]], writes=[bACC[dc]])
                    ring.release()

        for ps_i in range(npass):
            t0 = ps_i * TP
            xv = xres.rearrange("(c p) t -> p c t", p=128)
            for q in range(4):
                P.dma("sync", "in", ACC[:, q * 8:(q + 1) * 8, :], xv[:, q * 8:(q + 1) * 8, t0:t0 + TP],
                      writes=bACC[q * 8:(q + 1) * 8])
            if with_proj:
                mv = mix.rearrange("(c p) t -> p c t", p=128)
                for q in range(4):
                    P.dma("gpsimd", "w", XB[:, q * 8:(q + 1) * 8, :], mv[:, q * 8:(q + 1) * 8, t0:t0 + TP],
                          writes=[bXB])
                scale_acc()
                for dc in range(DC):
                    tw, bw = ring.take()
                    Wv = tw[:].rearrange("p (c f) -> p c f", f=128)
                    py = PY[dc % 2]
                    for c in range(DC):
                        P.op("tensor", (lambda c=c, Wv=Wv, py=py: PE.matmul(PS[py][:], lhsT=Wv[:, c, :], rhs=XB[:, c, :],
                                                                              start=(c == 0), stop=(c == DC - 1))),
                             reads=[bw, bXB], writes=[bPS[py]])
                    ring.release()
                    P.op("vector", (lambda dc=dc, py=py: V.tensor_tensor(out=ACC[:, dc, :], in0=PS[py][:],
                                                                          in1=ACC[:, dc, :], op=ALU.add)),
                         reads=[bPS[py], bACC[dc]], writes=[bACC[dc]])
                layer_norm(0, 1)
            router()
            cast_acc_to_xb()
            scale_acc()
            experts()
            layer_norm(2, 3, store_t0=t0)
        stats = P.emit(es)
    return nc, stats


def tail_consts():
    sel = np.zeros((32, NE * 128), np.float32)
    for e in range(NE):
        sel[e, e * 128:(e + 1) * 128] = 1.0
    return dict(ident=np.eye(128, dtype=np.float32), sel=sel)


def pc(v):
    return np.ascontiguousarray(np.asarray(v, np.float32).reshape(DC, 128).T)


def build_mix0(S=SEQ, n_units=5, stage=3):
    nc = bass.Bass("TRN2", target_bir_lowering=False)
    dt = nc.dram_tensor
    NT = S // TP
    NB = S // 128
    xT = dt("xT", [D, S], F32, kind="ExternalInput").ap()
    wp = dt("wp", [D, 512], F32, kind="ExternalInput").ap()
    wqkv = dt("wqkv", [4, D, 384], F32, kind="ExternalInput").ap()
    wf = dt("wf", [128, DC * 4], F32, kind="ExternalInput").ap()
    smallc = dt("smallc", [128, 64], F32, kind="ExternalInput").ap()
    wpool = dt("wpool", [512, 512], F32, kind="ExternalInput").ap()
    masks = dt("masks", [4, 128, 512], F32, kind="ExternalInput").ap()
    tri_d = dt("tri", [128, 128], F32, kind="ExternalInput").ap()
    ident_d = dt("ident", [128, 128], F32, kind="ExternalInput").ap()
    aT = dt("aT", [512, S], F32, kind="ExternalOutput").ap()
    bT = dt("bT", [512, S], F32, kind="ExternalOutput").ap()

    es = contextlib.ExitStack()
    with es:
        def sb(name, shape, dtype):
            return es.enter_context(nc.sbuf_tensor(name, shape, dtype))

        XT = [sb("XT%d" % i, [128, DC, TP], BF16) for i in range(2)]
        WU = [sb("WU%d" % i, [128, DC, 512], BF16) for i in range(2)]
        WF = sb("WF", [128, DC, 4], BF16)
        SMALL = sb("SMALL", [128, 64], F32)
        BFR = SMALL[:, 0:4]
        PSC = SMALL[:, 4:8]
        PCO = SMALL[:, 8:12]
        PCR = SMALL[:, 12:28]
        WPOOL = sb("WPOOL", [128, 4, 512], BF16)
        MASK = sb("MASK", [128, 4, 512], BF16)
        TRI = sb("TRI", [128, 128], F32)
        IDENT = sb("IDENT", [128, 128], F32)
        ONESF = sb("ONESF", [128, 128], F32)
        ONESB = sb("ONESB", [128, 128], BF16)
        U = sb("U", [128, 4, 16 + TP], F32)
        S2 = sb("S2", [128, 16 + TP], F32)
        S4 = sb("S4", [128, 16 + TP], F32)
        S8 = sb("S8", [128, 16 + TP], F32)
        S16 = sb("S16", [128, 16 + TP], F32)
        PA = sb("PA", [128, TP], F32)
        PLB = sb("PLB", [128, 4, TP], BF16)
        AO = [sb("AO%d" % i, [128, TP], F32) for i in range(2)]
        QT = sb("QT", [128, S], BF16)
        KT = sb("KT", [128, S], BF16)
        VV = sb("VV", [128, NB, 128], BF16)
        SP = sb("SP", [128, NB], F32)
        SPX = sb("SPX", [128, NB], F32)
        CN = sb("CN", [128, NB], F32)
        FZ = sb("FZ", [128, 4], F32)
        NMQ = sb("NMQ", [1, S], BF16)
        PT = [sb("PT%d" % i, [128, TP], BF16) for i in range(2)]
        RS = sb("RS", [128, TP], F32)
        EXA = [S2[:, 0:TP], S4[:, 0:TP]]
        BO = [sb("BO%d" % i, [128, TP], F32) for i in range(2)]
        PS = [es.enter_context(nc.psum_tensor("ps%d" % i, [128, 512], F32)) for i in range(8)]
        es.enter_context(nc.Block())

        P = Prog(nc)
        V, A, PE, G = nc.vector, nc.scalar, nc.tensor, nc.gpsimd
        bXT = [Buf(), Buf()]
        bWU = [Buf(), Buf()]
        bC = Buf("consts")
        bU = Buf("u")
        bS = Buf("swork")
        bPA = Buf("pa")
        bPLB = Buf("plb")
        bAO = [Buf(), Buf()]
        bQT, bKT, bVV, bSP, bCN, bNMQ = Buf(), Buf(), Buf(), Buf(), Buf(), Buf()
        bPT = [Buf(), Buf()]
        bEXA = [Buf(), Buf()]
        bRS = Buf()
        bBO = [Buf(), Buf()]
        bPS = [Buf("ps%d" % i) for i in range(8)]
        PPROJ, PSC_, POT, PSM, PMISC = (0, 1), (2, 3), 4, 5, (6, 7)

        P.dma("gpsimd", "c", WF[:].rearrange("p c h -> p (c h)"), wf, writes=[bC])
        P.dma("sync", "c", SMALL[:], smallc, writes=[bC])
        P.dma("gpsimd", "c", WPOOL[:], wpool.rearrange("(c p) d -> p c d", p=128), writes=[bC])
        P.dma("gpsimd", "c", MASK[:], masks.rearrange("m p q -> p m q"), writes=[bC])
        P.dma("sync", "c", TRI[:], tri_d, writes=[bC])
        P.dma("sync", "c", IDENT[:], ident_d, writes=[bC])
        P.op("vector", lambda: V.memset(ONESF[:], 1.0), writes=[bC])
        P.op("vector", lambda: V.memset(ONESB[:], 1.0), writes=[bC])
        P.op("vector", lambda: V.memset(U[:], 0.0), writes=[bU])

        xv = xT.rearrange("(c p) t -> p c t", p=128)

        def load_x(i, slot):
            for q in range(4):
                P.dma("gpsimd", "x", XT[slot][:, q * 8:(q + 1) * 8, :], xv[:, q * 8:(q + 1) * 8, i * TP:(i + 1) * TP],
                      writes=[bXT[slot]])

        def load_w(unit, slot):
            if unit == 0:
                srcv = wp.rearrange("(c p) f -> p c f", p=128)
                for q in range(4):
                    P.dma("gpsimd", "x", WU[slot][:, q * 8:(q + 1) * 8, :], srcv[:, q * 8:(q + 1) * 8, :], writes=[bWU[slot]])
            else:
                srcv = wqkv[unit - 1].rearrange("(c p) f -> p c f", p=128)
                for q in range(4):
                    P.dma("gpsimd", "x", WU[slot][:, q * 8:(q + 1) * 8, 0:384], srcv[:, q * 8:(q + 1) * 8, :],
                          writes=[bWU[slot]])

        load_w(0, 0)
        load_x(0, 0)
        load_x(1, 1)
        xcount = 0

        for unit in range(n_units):
            ws = unit % 2
            W = WU[ws]
            for i in range(NT):
                xs_ = xcount % 2
                X = XT[xs_]
                if unit == 0:
                    for c4 in range(4):
                        pp = PPROJ[c4 % 2]
                        for c in range(DC):
                            P.op("tensor", (lambda c=c, c4=c4, pp=pp, X=X, W=W: PE.matmul(PS[pp][:], lhsT=W[:, c, c4 * 128:(c4 + 1) * 128],
                                                                                       rhs=X[:, c, :], start=(c == 0), stop=(c == DC - 1))),
                                 reads=[bWU[ws], bXT[xs_]], writes=[bPS[pp]])
                        P.op("scalar", (lambda c4=c4, pp=pp: A.copy(out=U[:, c4, 16:16 + TP], in_=PS[pp][:])),
                             reads=[bPS[pp]], writes=[bU])
                    for c4 in range(4):
                        u = U[:, c4, :]
                        L = 16 + TP
                        P.op("vector", (lambda u=u: V.tensor_tensor(out=S2[:, 1:L], in0=u[:, 1:L], in1=u[:, 0:L - 1], op=ALU.add)),
                             reads=[bU, bS], writes=[bS])
                        P.op("vector", lambda: V.tensor_tensor(out=S4[:, 3:L], in0=S2[:, 3:L], in1=S2[:, 1:L - 2], op=ALU.add),
                             reads=[bS], writes=[bS])
                        P.op("vector", lambda: V.tensor_tensor(out=S8[:, 7:L], in0=S4[:, 7:L], in1=S4[:, 3:L - 4], op=ALU.add),
                             reads=[bS], writes=[bS])
                        P.op("vector", lambda: V.tensor_tensor(out=S16[:, 15:L], in0=S8[:, 15:L], in1=S8[:, 7:L - 8], op=ALU.add),
                             reads=[bS], writes=[bS])
                        P.op("vector", lambda: V.tensor_scalar(out=PA[:], in0=S2[:, 16:L], scalar1=PCO[:, 0:1], scalar2=None, op0=ALU.mult),
                             reads=[bS, bC, bPA], writes=[bPA])
                        for wi, SW in ((1, S4), (2, S8), (3, S16)):
                            P.op("vector", (lambda wi=wi, SW=SW: V.scalar_tensor_tensor(out=PA[:], in0=SW[:, 16:L], scalar=PCO[:, wi:wi + 1],
                                                                                         in1=PA[:], op0=ALU.mult, op1=ALU.add)),
                                 reads=[bS, bC, bPA], writes=[bPA])
                        if i == 0:
                            P.op("vector", lambda: V.tensor_tensor(out=PA[:, 0:16], in0=PA[:, 0:16], in1=PCR, op=ALU.mult),
                                 reads=[bPA, bC], writes=[bPA])
                        P.op("vector", (lambda c4=c4, u=u: V.tensor_tensor(out=PLB[:, c4, :], in0=PA[:], in1=u[:, 16:L], op=ALU.subtract)),
                             reads=[bPA, bU], writes=[bPLB])
                        P.op("vector", (lambda u=u: V.tensor_copy(out=u[:, 0:16], in_=u[:, TP:TP + 16])),
                             reads=[bU, bPLB], writes=[bU])
                    for o4 in range(4):
                        pp = PSC_[o4 % 2]
                        for c4 in range(4):
                            P.op("tensor", (lambda c4=c4, o4=o4, pp=pp: PE.matmul(PS[pp][:], lhsT=WPOOL[:, c4, o4 * 128:(o4 + 1) * 128],
                                                                                    rhs=PLB[:, c4, :], start=(c4 == 0), stop=(c4 == 3))),
                                 reads=[bC, bPLB], writes=[bPS[pp]])
                        ab = o4 % 2
                        P.op("scalar", (lambda o4=o4, pp=pp, ab=ab: A.activation(out=AO[ab][:], in_=PS[pp][:], func=AF.Identity,
                                                                                  bias=0.0, scale=PSC[:, o4:o4 + 1])),
                             reads=[bPS[pp], bC], writes=[bAO[ab]])
                        P.dma("sync", "out", aT[o4 * 128:(o4 + 1) * 128, i * TP:(i + 1) * TP], AO[ab][:], reads=[bAO[ab]])
                else:
                    h = unit - 1
                    for kind, dst, bdst in ((0, QT, bQT), (1, KT, bKT)):
                        pp = PPROJ[kind]
                        for c in range(DC):
                            P.op("tensor", (lambda c=c, kind=kind, pp=pp, X=X, W=W: PE.matmul(PS[pp][:], lhsT=W[:, c, kind * 128:(kind + 1) * 128],
                                                                                           rhs=X[:, c, :], start=(c == 0), stop=(c == DC - 1))),
                                 reads=[bWU[ws], bXT[xs_]], writes=[bPS[pp]])
                        sc = HEAD_DIM ** -0.5 if kind == 0 else 1.0
                        P.op("scalar", (lambda dst=dst, pp=pp, sc=sc, i=i: A.activation(out=dst[:, i * TP:(i + 1) * TP], in_=PS[pp][:],
                                                                                         func=AF.Identity, bias=0.0, scale=sc)),
                             reads=[bPS[pp]], writes=[bdst])
                    pv = PSC_[0]
                    for tb in range(4):
                        for c in range(DC):
                            P.op("tensor", (lambda c=c, tb=tb, X=X, W=W: PE.matmul(PS[pv][:, tb * 128:(tb + 1) * 128], lhsT=X[:, c, tb * 128:(tb + 1) * 128],
                                                                               rhs=W[:, c, 256:384], start=(c == 0), stop=(c == DC - 1))),
                                 reads=[bWU[ws], bXT[xs_]], writes=[bPS[pv]])
                    P.op("vector", (lambda i=i: V.tensor_copy(out=VV[:, i * 4:(i + 1) * 4, :].rearrange("p b d -> p (b d)"), in_=PS[pv][:])),
                         reads=[bPS[pv]], writes=[bVV])
                    pf = PSC_[1]
                    for tb in range(4):
                        for c in range(DC):
                            P.op("tensor", (lambda c=c, tb=tb, X=X, h=h: PE.matmul(PS[pf][:, tb:tb + 1], lhsT=X[:, c, tb * 128:(tb + 1) * 128],
                                                                                    rhs=WF[:, c, h:h + 1], start=(c == 0), stop=(c == DC - 1))),
                                 reads=[bC, bXT[xs_]], writes=[bPS[pf]])
                    P.op("vector", (lambda h=h: V.tensor_scalar(out=FZ[:], in0=PS[pf][:, 0:4], scalar1=BFR[:, h:h + 1], scalar2=-60.0,
                                                                 op0=ALU.add, op1=ALU.max)),
                         reads=[bPS[pf], bC, bSP], writes=[bSP])
                    P.op("scalar", lambda: A.activation(out=FZ[:], in_=FZ[:], func=AF.Exp, scale=-1.0), reads=[bSP], writes=[bSP])
                    P.op("scalar", (lambda i=i: A.activation(out=SP[:, i * 4:(i + 1) * 4], in_=FZ[:], func=AF.Ln, bias=1.0, scale=1.0)),
                         reads=[bSP], writes=[bSP])
                xcount += 1
                nxt = xcount + 1
                if nxt < n_units * NT:
                    load_x(nxt % NT, nxt % 2)
                if i == 0 and unit + 1 < n_units:
                    load_w(unit + 1, (unit + 1) % 2)
            if unit == 0 or stage < 2:
                continue
            h = unit - 1
            P.op("vector", lambda: V.memset(SPX[:, 0:1], 0.0), reads=[bSP], writes=[bSP])
            P.op("vector", lambda: V.tensor_tensor_scan(out=SPX[:, 1:NB], data0=ONESF[:, 0:NB - 1], data1=SP[:, 0:NB - 1], initial=0.0,
                                                        op0=ALU.mult, op1=ALU.add), reads=[bSP, bC], writes=[bSP])
            pm = PMISC[0]
            P.op("tensor", lambda: PE.matmul(PS[pm][:, 0:NB], lhsT=TRI[:], rhs=SP[:], start=True, stop=False),
                 reads=[bC, bSP], writes=[bPS[pm]])
            P.op("tensor", lambda: PE.matmul(PS[pm][:, 0:NB], lhsT=ONESF[:], rhs=SPX[:], start=False, stop=True),
                 reads=[bC, bSP], writes=[bPS[pm]])
            P.op("vector", lambda: V.tensor_copy(out=CN[:], in_=PS[pm][:, 0:NB]), reads=[bPS[pm]], writes=[bCN])
            pr = PMISC[1]
            for g in range(NT):
                for tb in range(4):
                    kb = g * 4 + tb
                    P.op("tensor", (lambda kb=kb, tb=tb: PE.matmul(PS[pr][0:1, tb * 128:(tb + 1) * 128], lhsT=CN[:, kb:kb + 1], rhs=IDENT[:],
                                                                    start=True, stop=True)),
                         reads=[bCN, bC], writes=[bPS[pr]])
                P.op("scalar", (lambda g=g: A.activation(out=NMQ[0:1, g * TP:(g + 1) * TP], in_=PS[pr][0:1, :], func=AF.Identity,
                                                          bias=0.0, scale=-1.0)),
                     reads=[bPS[pr]], writes=[bNMQ])
            if stage < 3:
                continue
            items = [(g, kk) for g in range(NT) for kk in range(4 * g + 4)]

            def emit_scores(idx):
                g, kk = items[idx]
                psb = PSC_[idx % 2]
                P.op("tensor", (lambda kk=kk, g=g, psb=psb: PE.matmul(PS[psb][:], lhsT=KT[:, kk * 128:(kk + 1) * 128], rhs=QT[:, g * TP:(g + 1) * TP],
                                                                       start=True, stop=False)),
                     reads=[bKT, bQT], writes=[bPS[psb]])
                P.op("tensor", (lambda g=g, psb=psb: PE.matmul(PS[psb][:], lhsT=ONESB[0:1, :], rhs=NMQ[0:1, g * TP:(g + 1) * TP],
                                                                start=False, stop=True)),
                     reads=[bC, bNMQ], writes=[bPS[psb]])

            emit_scores(0)
            for idx, (g, kk) in enumerate(items):
                nk = 4 * g + 4
                psb = PSC_[idx % 2]
                pb = idx % 2
                if kk >= 4 * g:
                    P.op("vector", (lambda kk=kk, psb=psb, pb=pb: V.tensor_scalar(out=EXA[pb], in0=PS[psb][:], scalar1=CN[:, kk:kk + 1],
                                                                                   scalar2=60.0, op0=ALU.add, op1=ALU.min)),
                         reads=[bPS[psb], bCN], writes=[bEXA[pb]])
                    P.op("scalar", (lambda pb=pb: A.activation(out=PT[pb][:], in_=EXA[pb], func=AF.Exp)),
                         reads=[bEXA[pb]], writes=[bPT[pb]])
                    P.op("vector", (lambda kk=kk, g=g, pb=pb: V.tensor_tensor(out=PT[pb][:], in0=PT[pb][:], in1=MASK[:, kk - 4 * g, :], op=ALU.mult)),
                         reads=[bPT[pb], bC], writes=[bPT[pb]])
                else:
                    P.op("scalar", (lambda kk=kk, psb=psb, pb=pb: A.activation(out=PT[pb][:], in_=PS[psb][:], func=AF.Exp,
                                                                                bias=CN[:, kk:kk + 1], scale=1.0)),
                         reads=[bPS[psb], bCN], writes=[bPT[pb]])
                if idx + 1 < len(items):
                    emit_scores(idx + 1)
                P.op("tensor", (lambda kk=kk, pb=pb, nk=nk: PE.matmul(PS[POT][:], lhsT=VV[:, kk, :], rhs=PT[pb][:], start=(kk == 0), stop=(kk == nk - 1))),
                     reads=[bVV, bPT[pb]], writes=[bPS[POT]])
                P.op("tensor", (lambda kk=kk, pb=pb, nk=nk: PE.matmul(PS[PSM][:], lhsT=ONESB[:], rhs=PT[pb][:], start=(kk == 0), stop=(kk == nk - 1))),
                     reads=[bC, bPT[pb]], writes=[bPS[PSM]])
                if kk == nk - 1:
                    P.op("vector", lambda: V.reciprocal(out=RS[:], in_=PS[PSM][:]), reads=[bPS[PSM], bRS], writes=[bRS])
                    ob = g % 2
                    P.op("vector", (lambda ob=ob: V.tensor_tensor(out=BO[ob][:], in0=PS[POT][:], in1=RS[:], op=ALU.mult)),
                         reads=[bPS[POT], bRS], writes=[bBO[ob]])
                    P.dma("sync", "out", bT[h * 128:(h + 1) * 128, g * TP:(g + 1) * TP], BO[ob][:], reads=[bBO[ob]])
        stats = P.emit(es)
    return nc, stats


def mix0_consts():
    masks = np.zeros((4, 128, 512), np.float32)
    kl = np.arange(128)[:, None]
    ql = np.arange(512)[None, :]
    for m in range(4):
        masks[m] = (128 * m + kl <= ql).astype(np.float32)
    s = np.arange(128)
    tri = (s[:, None] <= s[None, :]).astype(np.float32)
    return dict(masks=masks, tri=tri, ident=np.eye(128, dtype=np.float32))


GELU_C = 0.7978845608028654


def build_mix1(S=SEQ, n_units=4):
    nc = bass.Bass("TRN2", target_bir_lowering=False)
    dt = nc.dram_tensor
    NT = S // TP
    xT = dt("xT", [D, S], F32, kind="ExternalInput").ap()
    win = dt("win", [4, D, 512], F32, kind="ExternalInput").ap()
    wa = dt("wa", [4, 256, 256], F32, kind="ExternalInput").ap()
    wx = dt("wx", [4, 256, 256], F32, kind="ExternalInput").ap()
    smallc = dt("smallc", [128, 64], F32, kind="ExternalInput").ap()
    yT = dt("yT", [1024, S], F32, kind="ExternalOutput").ap()

    es = contextlib.ExitStack()
    with es:
        def sb(name, shape, dtype):
            return es.enter_context(nc.sbuf_tensor(name, shape, dtype))

        XT = [sb("XT%d" % i, [128, DC, TP], BF16) for i in range(2)]
        WU = [sb("WU%d" % i, [128, DC, 512], BF16) for i in range(2)]
        WA = sb("WA", [128, 8, 256], BF16)
        WX = sb("WX", [128, 8, 256], BF16)
        SMALL = sb("SMALL", [128, 64], F32)
        SMV = SMALL[:].rearrange("p (n j) -> p n j", j=8)
        LAB = {n: sb("lab_" + n, [128, 8], F32) for n in ("u", "lnv", "ser", "msk", "a8", "a16")}
        ONESF = sb("ONESF", [128, 8], F32)

        def pair(name, dtype=F32, w=TP):
            return [sb("%s%d" % (name, i), [128, w], dtype) for i in range(2)]

        T1, GATE, XC, R, IG, AA, A2, BT, H, Y = [pair(n) for n in
                                                ("T1", "GATE", "XC", "R", "IG", "AA", "A2", "BT", "H", "Y")]
        GXp = [pair("GXa"), pair("GXb")]
        XBUFp = [pair("XBUFa", F32, TP + 3), pair("XBUFb", F32, TP + 3)]
        XCB = pair("XCB", BF16)
        HS = sb("HS", [128, 2], F32)
        PS = [es.enter_context(nc.psum_tensor("ps%d" % i, [128, 512], F32)) for i in range(8)]
        es.enter_context(nc.Block())

        P = Prog(nc)
        V, A, PE, G = nc.vector, nc.scalar, nc.tensor, nc.gpsimd
        bXT = [Buf(), Buf()]
        bWU = [Buf(), Buf()]
        bC = Buf("consts")
        bLAB = Buf("lab")
        bk = lambda: [Buf(), Buf()]
        bT1, bGATE, bXC, bR, bIG, bAA, bA2, bBT, bH, bY, bXCB, bHS = [bk() for _ in range(12)]
        bGXp = [bk(), bk()]
        bXBUFp = [bk(), bk()]
        bPS = [Buf("ps%d" % i) for i in range(8)]
        PPROJ, PGA, PGX = (0, 1, 2, 3), (4, 5), (6, 7)

        P.dma("sync", "c", SMALL[:], smallc, writes=[bC])
        P.dma("gpsimd", "c", WA[:].rearrange("p (u k) j -> p u k j", k=2), wa.rearrange("u (k p) j -> p u k j", p=128), writes=[bC])
        P.dma("gpsimd", "c", WX[:].rearrange("p (u k) j -> p u k j", k=2), wx.rearrange("u (k p) j -> p u k j", p=128), writes=[bC])
        P.op("vector", lambda: V.memset(ONESF[:], 1.0), writes=[bC])

        lam = SMV[:, :, 7]
        lb = [bLAB]
        P.op("scalar", lambda: A.activation(out=LAB["u"][:], in_=lam, func=AF.Exp, scale=-1.0), reads=[bC], writes=lb)
        P.op("scalar", lambda: A.activation(out=LAB["lnv"][:], in_=LAB["u"][:], func=AF.Ln, bias=1.0, scale=1.0), reads=lb, writes=lb)
        P.op("vector", lambda: V.tensor_scalar(out=LAB["ser"][:], in0=LAB["u"][:], scalar1=-0.25, scalar2=1.0 / 3.0, op0=ALU.mult, op1=ALU.add),
             reads=lb, writes=lb)
        P.op("vector", lambda: V.tensor_tensor(out=LAB["ser"][:], in0=LAB["ser"][:], in1=LAB["u"][:], op=ALU.mult), reads=lb, writes=lb)
        P.op("vector", lambda: V.tensor_scalar(out=LAB["ser"][:], in0=LAB["ser"][:], scalar1=-1.0, scalar2=0.5, op0=ALU.mult, op1=ALU.add),
             reads=lb, writes=lb)
        P.op("vector", lambda: V.tensor_tensor(out=LAB["ser"][:], in0=LAB["ser"][:], in1=LAB["u"][:], op=ALU.mult), reads=lb, writes=lb)
        P.op("vector", lambda: V.tensor_scalar(out=LAB["ser"][:], in0=LAB["ser"][:], scalar1=-1.0, scalar2=1.0, op0=ALU.mult, op1=ALU.add),
             reads=lb, writes=lb)
        P.op("vector", lambda: V.tensor_tensor(out=LAB["ser"][:], in0=LAB["ser"][:], in1=LAB["u"][:], op=ALU.mult), reads=lb, writes=lb)
        P.op("vector", lambda: V.tensor_scalar(out=LAB["msk"][:], in0=LAB["u"][:], scalar1=0.05, scalar2=None, op0=ALU.is_lt), reads=lb, writes=lb)
        P.op("vector", lambda: V.tensor_tensor(out=LAB["ser"][:], in0=LAB["ser"][:], in1=LAB["lnv"][:], op=ALU.subtract), reads=lb, writes=lb)
        P.op("vector", lambda: V.tensor_tensor(out=LAB["ser"][:], in0=LAB["ser"][:], in1=LAB["msk"][:], op=ALU.mult), reads=lb, writes=lb)
        P.op("vector", lambda: V.tensor_tensor(out=LAB["ser"][:], in0=LAB["ser"][:], in1=LAB["lnv"][:], op=ALU.add), reads=lb, writes=lb)
        P.op("vector", lambda: V.tensor_scalar(out=LAB["a8"][:], in0=LAB["ser"][:], scalar1=-LRU_C, scalar2=None, op0=ALU.mult), reads=lb, writes=lb)
        P.op("vector", lambda: V.tensor_scalar(out=LAB["a16"][:], in0=LAB["ser"][:], scalar1=-2.0 * LRU_C, scalar2=None, op0=ALU.mult),
             reads=lb, writes=lb)

        xv = xT.rearrange("(c p) t -> p c t", p=128)

        def load_x(i, slot):
            for q in range(4):
                P.dma("gpsimd", "x", XT[slot][:, q * 8:(q + 1) * 8, :], xv[:, q * 8:(q + 1) * 8, i * TP:(i + 1) * TP],
                      writes=[bXT[slot]])

        def load_w(unit, slot):
            srcv = win[unit].rearrange("(c p) f -> p c f", p=128)
            for q in range(4):
                P.dma("gpsimd", "x", WU[slot][:, q * 8:(q + 1) * 8, :], srcv[:, q * 8:(q + 1) * 8, :], writes=[bWU[slot]])

        load_w(0, 0)
        load_x(0, 0)
        load_x(1, 1)
        items = [(u, i) for u in range(n_units) for i in range(NT)]

        def stage_a(n):
            unit, i = items[n]
            ws, xs_, par = unit % 2, n % 2, n % 2
            W, X = WU[ws], XT[xs_]
            for oc in range(4):
                pp = PPROJ[oc]
                for c in range(DC):
                    P.op("tensor", (lambda c=c, oc=oc, pp=pp, X=X, W=W: PE.matmul(PS[pp][:], lhsT=W[:, c, oc * 128:(oc + 1) * 128],
                                                                                   rhs=X[:, c, :], start=(c == 0), stop=(c == DC - 1))),
                         reads=[bWU[ws], bXT[xs_]], writes=[bPS[pp]])
                k = oc % 2
                if oc < 2:
                    P.op("scalar", (lambda k=k, pp=pp, par=par: A.copy(out=GXp[par][k][:], in_=PS[pp][:])),
                         reads=[bPS[pp]], writes=[bGXp[par][k]])
                else:
                    P.op("scalar", (lambda k=k, pp=pp, par=par: A.copy(out=XBUFp[par][k][:, 3:3 + TP], in_=PS[pp][:])),
                         reads=[bPS[pp]], writes=[bXBUFp[par][k]])
                    if i == 0:
                        P.op("vector", (lambda k=k, par=par: V.memset(XBUFp[par][k][:, 0:3], 0.0)),
                             reads=[bXBUFp[par][k]], writes=[bXBUFp[par][k]])
                    else:
                        P.op("vector", (lambda k=k, par=par: V.tensor_copy(out=XBUFp[par][k][:, 0:3], in_=XBUFp[1 - par][k][:, TP:TP + 3])),
                             reads=[bXBUFp[1 - par][k], bXBUFp[par][k]], writes=[bXBUFp[par][k]])
            nxt = n + 2
            if nxt < len(items):
                load_x(items[nxt][1], nxt % 2)
            if i == 0 and unit + 1 < n_units:
                load_w(unit + 1, (unit + 1) % 2)

        def stage_b(n):
            unit, i = items[n]
            par = n % 2
            for k in range(2):
                GXk = GXp[par][k]
                P.op("vector", (lambda k=k, GXk=GXk: V.tensor_tensor(out=T1[k][:], in0=GXk[:], in1=GXk[:], op=ALU.mult)),
                     reads=[bGXp[par][k], bT1[k]], writes=[bT1[k]])
                P.op("vector", (lambda k=k: V.tensor_scalar(out=T1[k][:], in0=T1[k][:], scalar1=2.0 * GELU_C * 0.044715, scalar2=2.0 * GELU_C,
                                                             op0=ALU.mult, op1=ALU.add)), reads=[bT1[k]], writes=[bT1[k]])
                P.op("vector", (lambda k=k, GXk=GXk: V.tensor_tensor(out=T1[k][:], in0=T1[k][:], in1=GXk[:], op=ALU.mult)),
                     reads=[bT1[k], bGXp[par][k]], writes=[bT1[k]])
                P.op("scalar", (lambda k=k: A.activation(out=T1[k][:], in_=T1[k][:], func=AF.Sigmoid)), reads=[bT1[k]], writes=[bT1[k]])
                P.op("gpsimd", (lambda k=k, GXk=GXk: G.tensor_tensor(out=GATE[k][:], in0=T1[k][:], in1=GXk[:], op=ALU.mult)),
                     reads=[bT1[k], bGXp[par][k], bGATE[k]], writes=[bGATE[k]])
            for k in range(2):
                nn = unit * 2 + k
                xb = XBUFp[par][k]
                P.op("vector", (lambda k=k, nn=nn, xb=xb: V.tensor_scalar(out=XC[k][:], in0=xb[:, 3:3 + TP], scalar1=SMV[:, nn, 3:4], scalar2=SMV[:, nn, 4:5],
                                                                           op0=ALU.mult, op1=ALU.add)),
                     reads=[bXBUFp[par][k], bC, bXC[k]], writes=[bXC[k]])
                for tap in (2, 1, 0):
                    P.op("vector", (lambda k=k, nn=nn, xb=xb, tap=tap: V.scalar_tensor_tensor(out=XC[k][:], in0=xb[:, tap:tap + TP], scalar=SMV[:, nn, tap:tap + 1],
                                                                                               in1=XC[k][:], op0=ALU.mult, op1=ALU.add)),
                         reads=[bXBUFp[par][k], bC, bXC[k]], writes=[bXC[k]])
                P.op("scalar", (lambda k=k: A.copy(out=XCB[k][:], in_=XC[k][:])), reads=[bXC[k]], writes=[bXCB[k]])

        def stage_c(n):
            unit, i = items[n]
            if i == 0:
                for k in range(2):
                    P.op("vector", (lambda k=k: V.memset(HS[:, k:k + 1], 0.0)), reads=[bHS[k]], writes=[bHS[k]])
            for j in range(2):
                nn = unit * 2 + j
                for (Wg, pg, dst, bdst, bcol) in ((WA, PGA[j], R, bR, 5), (WX, PGX[j], IG, bIG, 6)):
                    for k in range(2):
                        P.op("tensor", (lambda k=k, j=j, Wg=Wg, pg=pg, unit=unit: PE.matmul(PS[pg][:], lhsT=Wg[:, unit * 2 + k, j * 128:(j + 1) * 128],
                                                                                             rhs=XCB[k][:], start=(k == 0), stop=(k == 1))),
                             reads=[bC, bXCB[0], bXCB[1]], writes=[bPS[pg]])
                    P.op("scalar", (lambda j=j, nn=nn, pg=pg, dst=dst, bcol=bcol: A.activation(out=dst[j][:], in_=PS[pg][:], func=AF.Sigmoid,
                                                                                                bias=SMV[:, nn, bcol:bcol + 1], scale=1.0)),
                         reads=[bPS[pg], bC], writes=[bdst[j]])
            for j in range(2):
                nn = unit * 2 + j
                P.op("scalar", (lambda j=j, nn=nn: A.activation(out=AA[j][:], in_=R[j][:], func=AF.Exp, scale=LAB["a8"][:, nn:nn + 1])),
                     reads=[bR[j], bLAB], writes=[bAA[j]])
                P.op("scalar", (lambda j=j, nn=nn: A.activation(out=A2[j][:], in_=R[j][:], func=AF.Exp, scale=LAB["a16"][:, nn:nn + 1])),
                     reads=[bR[j], bLAB], writes=[bA2[j]])
            for j in range(2):
                P.op("vector", (lambda j=j: V.tensor_scalar(out=A2[j][:], in0=A2[j][:], scalar1=-1.0, scalar2=1.0, op0=ALU.mult, op1=ALU.add)),
                     reads=[bA2[j]], writes=[bA2[j]])
                P.op("vector", (lambda j=j: V.tensor_scalar(out=A2[j][:], in0=A2[j][:], scalar1=0.0, scalar2=None, op0=ALU.max)),
                     reads=[bA2[j]], writes=[bA2[j]])
            for j in range(2):
                P.op("scalar", (lambda j=j: A.activation(out=A2[j][:], in_=A2[j][:], func=AF.Sqrt)), reads=[bA2[j]], writes=[bA2[j]])
            for j in range(2):
                P.op("vector", (lambda j=j: V.tensor_tensor(out=BT[j][:], in0=A2[j][:], in1=IG[j][:], op=ALU.mult)),
                     reads=[bA2[j], bIG[j], bBT[j]], writes=[bBT[j]])
                P.op("vector", (lambda j=j: V.tensor_tensor(out=BT[j][:], in0=BT[j][:], in1=XC[j][:], op=ALU.mult)),
                     reads=[bBT[j], bXC[j]], writes=[bBT[j]])
                P.op("vector", (lambda j=j: V.tensor_tensor_scan(out=H[j][:], data0=AA[j][:], data1=BT[j][:], initial=HS[:, j:j + 1],
                                                                  op0=ALU.mult, op1=ALU.add)),
                     reads=[bAA[j], bBT[j], bHS[j], bH[j]], writes=[bH[j]])
                P.op("vector", (lambda j=j: V.tensor_copy(out=HS[:, j:j + 1], in_=H[j][:, TP - 1:TP])), reads=[bH[j]], writes=[bHS[j]])
                P.op("gpsimd", (lambda j=j: G.tensor_tensor(out=Y[j][:], in0=H[j][:], in1=GATE[j][:], op=ALU.mult)),
                     reads=[bH[j], bGATE[j], bY[j]], writes=[bY[j]])
                row = (unit * 2 + j) * 128
                P.dma("sync", "out", yT[row:row + 128, i * TP:(i + 1) * TP], Y[j][:], reads=[bY[j]])

        stage_a(0)
        for n in range(len(items)):
            stage_b(n)
            if n + 1 < len(items):
                stage_a(n + 1)
            stage_c(n)
        stats = P.emit(es)
    return nc, stats


_PROGS = {}


def _prog(name):
    if name not in _PROGS:
        if name == "tail":
            _PROGS[name] = build_tail(SEQ // 4, NE, True)[0]
        elif name == "mix0":
            _PROGS[name] = build_mix0(SEQ)[0]
        elif name == "mix1":
            _PROGS[name] = build_mix1(SEQ)[0]
    return _PROGS[name]


def _c(a):
    return np.ascontiguousarray(a, dtype=np.float32)


def _mix0_inputs(c, xT_b, w_in, w_pool, pscale, b_f, consts):
    g = c % 4
    heads = [4 * g + i for i in range(4)]
    wqkv = np.stack([np.concatenate([w_in[:, 2048 + h * 128:2048 + (h + 1) * 128],
                                     w_in[:, 4096 + h * 128:4096 + (h + 1) * 128],
                                     w_in[:, 6144 + h * 128:6144 + (h + 1) * 128]], 1) for h in heads])
    wf = w_in[:, 8192 + 4 * g:8192 + 4 * g + 4]
    wf_l = wf.reshape(DC, 128, 4).transpose(1, 0, 2).reshape(128, DC * 4)
    small = np.zeros((128, 64), np.float32)
    small[:, 0:4] = b_f[4 * g:4 * g + 4][None]
    small[:, 4:8] = pscale[g * 512:(g + 1) * 512].reshape(4, 128).T
    w = 2 ** (g + 1)
    small[:, 8 + g] = 1.0 / w
    small[:, 12:28] = (w / np.minimum(np.arange(16) + 1, w))[None]
    return dict(xT=xT_b, wp=_c(w_in[:, g * 512:(g + 1) * 512]), wqkv=_c(wqkv), wf=_c(wf_l), smallc=small,
                wpool=_c(w_pool[g]), **consts)


def _mix1_inputs(c, xT_b, w_in, conv_w, conv_b, w_a, b_a, w_x, b_x, lam):
    qt = c % 4
    ch0 = qt * 1024
    win = np.stack([np.concatenate([w_in[:, ch0 + u * 256:ch0 + (u + 1) * 256],
                                    w_in[:, D + ch0 + u * 256:D + ch0 + (u + 1) * 256]], 1) for u in range(4)])
    small = np.zeros((128, 8, 8), np.float32)
    b_a = b_a.reshape(-1)
    b_x = b_x.reshape(-1)
    for n in range(8):
        sl = slice(ch0 + n * 128, ch0 + (n + 1) * 128)
        for i in range(4):
            small[:, n, i] = conv_w[i, sl]
        small[:, n, 4] = conv_b[sl]
        small[:, n, 5] = b_a[sl]
        small[:, n, 6] = b_x[sl]
        small[:, n, 7] = lam[sl]
    return dict(xT=xT_b, win=_c(win), wa=_c(w_a[qt * 4:qt * 4 + 4]), wx=_c(w_x[qt * 4:qt * 4 + 4]),
                smallc=small.reshape(128, 64))


def _tail_inputs(layer, xresT, mixT, wout, moe_w_group, moe_b_group, moe_w_expert, moe_b_expert,
                 moe_w1, moe_w3, moe_w2, ln_g, ln_b, consts):
    wr = _c(np.concatenate([moe_w_group[layer], moe_w_expert[layer]], 1))
    br = _c(np.tile(np.concatenate([moe_b_group[layer], moe_b_expert[layer]])[None], (128, 1)))
    lnp = _c(np.stack([pc(ln_g[layer, 0]), pc(ln_b[layer, 0]), pc(ln_g[layer, 1]), pc(ln_b[layer, 1])], 1))
    common = dict(wr=wr, br=br, w1=_c(moe_w1[layer]), w3=_c(moe_w3[layer]), w2=_c(moe_w2[layer]),
                  lnp=lnp, wout=_c(wout), **consts)
    return [dict(xres=xresT[c], mix=mixT[c], **common) for c in range(8)]


def kernel(x, even_w_in, even_w_pool, even_pool_scale, even_b_f, even_w_out,
           odd_w_in, odd_conv_w, odd_conv_b, odd_w_a, odd_b_a, odd_w_x, odd_b_x,
           odd_lambda, odd_w_out, moe_w_group, moe_b_group, moe_w_expert, moe_b_expert,
           moe_w1, moe_w3, moe_w2, ln_g, ln_b):
    f = lambda a: np.asarray(a, dtype=np.float32)
    x = f(x)
    cores = list(range(8))
    TQ = SEQ // 4
    xT = [_c(x[b].T) for b in range(BATCH)]

    c0 = mix0_consts()
    in0 = [_mix0_inputs(c, xT[c // 4], f(even_w_in[0]), f(even_w_pool[0]), f(even_pool_scale[0]), f(even_b_f[0]), c0)
           for c in cores]
    r0 = run_bass_kernel_spmd(_prog("mix0"), in0, core_ids=cores).results
    mixT = []
    for c in cores:
        b, j = c // 4, c % 4
        ts = slice(j * TQ, (j + 1) * TQ)
        mixT.append(_c(np.concatenate([r0[4 * b + g]["aT"][:, ts] for g in range(4)] +
                                      [r0[4 * b + g]["bT"][:, ts] for g in range(4)], 0)))
    xres = [_c(xT[c // 4][:, (c % 4) * TQ:(c % 4 + 1) * TQ]) for c in cores]
    tc = tail_consts()
    moe = (f(moe_w_group), f(moe_b_group), f(moe_w_expert), f(moe_b_expert), moe_w1, moe_w3, moe_w2, f(ln_g), f(ln_b))
    r1 = run_bass_kernel_spmd(_prog("tail"), _tail_inputs(0, xres, mixT, f(even_w_out[0]), *moe, tc), core_ids=cores).results
    x2 = [r1[c]["xout"] for c in cores]

    x2T = [_c(np.concatenate([x2[4 * b + j] for j in range(4)], 1)) for b in range(BATCH)]
    in2 = [_mix1_inputs(c, x2T[c // 4], f(odd_w_in[0]), f(odd_conv_w[0]), f(odd_conv_b[0]), f(odd_w_a[0]), f(odd_b_a[0]),
                        f(odd_w_x[0]), f(odd_b_x[0]), f(odd_lambda[0])) for c in cores]
    r2 = run_bass_kernel_spmd(_prog("mix1"), in2, core_ids=cores).results
    mixT = []
    for c in cores:
        b, j = c // 4, c % 4
        ts = slice(j * TQ, (j + 1) * TQ)
        mixT.append(_c(np.concatenate([r2[4 * b + q]["yT"][:, ts] for q in range(4)], 0)))
    r3 = run_bass_kernel_spmd(_prog("tail"), _tail_inputs(1, x2, mixT, f(odd_w_out[0]), *moe, tc), core_ids=cores).results
    out = np.empty((BATCH, SEQ, D), np.float32)
    for c in cores:
        b, j = c // 4, c % 4
        out[b, j * TQ:(j + 1) * TQ, :] = r3[c]["xout"].T
    return out
```

```python
import contextlib
import numpy as np
import concourse.bass as bass
import concourse.mybir as mybir
from concourse.bass_utils import run_bass_kernel_spmd

F32 = mybir.dt.float32
BF16 = mybir.dt.bfloat16
AF = mybir.ActivationFunctionType
ALU = mybir.AluOpType
AX = mybir.AxisListType

D = 4096
DC = 32
SEQ = 4096
BATCH = 2
TP = 512
NE = 32
FF = 512
ALPHA = 4.0 ** 0.25
EPS = 1e-5
BIG = 1.0e4
NSLOT = 8
SEM_LIMIT = 30000
HEAD_DIM = 128
LRU_C = 8.0


class Buf:
    __slots__ = ("name", "w", "r")

    def __init__(self, name=""):
        self.name = name
        self.w = None
        self.r = {}


class Op:
    __slots__ = ("lane", "fn", "deps", "idx", "signal", "sigval")

    def __init__(self, lane, fn):
        self.lane = lane
        self.fn = fn
        self.deps = {}
        self.idx = 0
        self.signal = False
        self.sigval = None


class Lane:
    def __init__(self, name, issuer, inc, is_dma):
        self.name = name
        self.issuer = issuer
        self.inc = inc
        self.is_dma = is_dma
        self.ops = []


class Prog:
    def __init__(self, nc):
        self.nc = nc
        self.ops = []
        self.lanes = {}
        for n in ("tensor", "vector", "scalar", "gpsimd"):
            self.lanes[n] = Lane(n, n, 1, False)
        self.eng = {"tensor": nc.tensor, "vector": nc.vector, "scalar": nc.scalar,
                    "gpsimd": nc.gpsimd, "sync": nc.sync}

    def dma_lane(self, issuer, cls):
        key = "dma_%s_%s" % (issuer, cls)
        if key not in self.lanes:
            self.lanes[key] = Lane(key, issuer, 16, True)
        return key

    def op(self, lane, fn, reads=(), writes=()):
        ln = self.lanes[lane]
        o = Op(ln, fn)
        o.idx = len(ln.ops) + 1
        ln.ops.append(o)
        deps = o.deps

        def add(d, war=False):
            if d is None:
                return
            if d.lane is ln and not ln.is_dma:
                if ln.name == "tensor" or war:
                    return
            cur = deps.get(d.lane.name)
            if cur is None or cur.idx < d.idx:
                deps[d.lane.name] = d

        for b in reads:
            add(b.w)
        for b in writes:
            add(b.w)
            for rd in b.r.values():
                add(rd, war=True)
        for b in reads:
            b.r[ln.name] = o
        for b in writes:
            b.w = o
            b.r = {}
        self.ops.append(o)
        return o

    def dma(self, issuer, cls, out, in_, reads=(), writes=()):
        lane = self.dma_lane(issuer, cls)
        eng = self.eng[issuer]
        return self.op(lane, lambda: eng.dma_start(out=out, in_=in_), reads, writes)

    def emit(self, es):
        nc = self.nc
        waited = {}
        plan = []
        for o in self.ops:
            wd = waited.setdefault(o.lane.issuer, {})
            waits = []
            for lname, d in o.deps.items():
                if wd.get(lname, 0) >= d.idx:
                    continue
                waits.append(d)
                wd[lname] = d.idx
            plan.append(waits)
        for waits in plan:
            for d in waits:
                d.signal = True
        final_lanes = [l.name for l in self.lanes.values() if l.is_dma and l.ops]
        for lname in final_lanes:
            self.lanes[lname].ops[-1].signal = True
        sems = {}

        def get_sem(lname, epoch):
            k = (lname, epoch)
            if k not in sems:
                sems[k] = es.enter_context(nc.semaphore("s_%s_%d" % (lname, epoch)))
            return sems[k]

        for ln in self.lanes.values():
            cnt = 0
            epoch = 0
            for o in ln.ops:
                if ln.is_dma or o.signal:
                    o.signal = True
                    if cnt + ln.inc > SEM_LIMIT:
                        epoch += 1
                        cnt = 0
                    cnt += ln.inc
                    o.sigval = (epoch, cnt)
        nwaits = 0
        for o, waits in zip(self.ops, plan):
            eng = self.eng[o.lane.issuer]
            for d in waits:
                ep, v = d.sigval
                eng.wait_ge(get_sem(d.lane.name, ep), v)
                nwaits += 1
            ins = o.fn()
            if o.signal:
                ep, v = o.sigval
                ins.then_inc(get_sem(o.lane.name, ep), o.lane.inc)
        for lname in final_lanes:
            ep, v = self.lanes[lname].ops[-1].sigval
            nc.sync.wait_ge(get_sem(lname, ep), v)
        return dict(n_ops=len(self.ops), n_waits=nwaits, n_sems=len(sems))


class Ring:
    def __init__(self, P, tiles, bufs):
        self.P = P
        self.tiles = tiles
        self.bufs = bufs
        self.n = len(tiles)
        self.queue = []
        self.loaded = 0
        self.used = 0
        self.released = 0

    def push(self, loader):
        self.queue.append(loader)

    def _pump(self):
        while self.loaded < len(self.queue) and self.loaded < self.released + self.n:
            s = self.loaded % self.n
            self.queue[self.loaded](self.tiles[s], self.bufs[s])
            self.loaded += 1

    def take(self):
        if self.used >= self.loaded:
            self._pump()
        assert self.used < self.loaded
        s = self.used % self.n
        self.used += 1
        return self.tiles[s], self.bufs[s]

    def release(self, k=1):
        self.released += k
        assert self.released <= self.used
        self._pump()


def build_tail(T=1024, n_exp=NE, with_proj=True):
    nc = bass.Bass("TRN2", target_bir_lowering=False)
    dt = nc.dram_tensor
    xres = dt("xres", [D, T], F32, kind="ExternalInput").ap()
    if with_proj:
        mix = dt("mix", [D, T], F32, kind="ExternalInput").ap()
        wout = dt("wout", [D, D], F32, kind="ExternalInput").ap()
    wr = dt("wr", [D, 36], F32, kind="ExternalInput").ap()
    br = dt("br", [128, 36], F32, kind="ExternalInput").ap()
    w1 = dt("w1", [n_exp, D, FF], F32, kind="ExternalInput").ap()
    w3 = dt("w3", [n_exp, D, FF], F32, kind="ExternalInput").ap()
    w2 = dt("w2", [n_exp, FF, D], F32, kind="ExternalInput").ap()
    lnp = dt("lnp", [128, 4, DC], F32, kind="ExternalInput").ap()
    ident_d = dt("ident", [128, 128], F32, kind="ExternalInput").ap()
    sel_d = dt("sel", [32, NE * 128], F32, kind="ExternalInput").ap()
    xout = dt("xout", [D, T], F32, kind="ExternalOutput").ap()

    es = contextlib.ExitStack()
    with es:
        def sb(name, shape, dtype):
            return es.enter_context(nc.sbuf_tensor(name, shape, dtype))

        ACC = sb("ACC", [128, DC, TP], F32)
        XB = sb("XB", [128, DC, TP], BF16)
        RINGT = [sb("ring%d" % i, [128, 4096], BF16) for i in range(NSLOT)]
        HT = [sb("HT%d" % i, [128, 4, TP], BF16) for i in range(2)]
        S = [sb("S%d" % i, [128, TP], F32) for i in range(4)]
        TT = [sb("TT%d" % i, [128, TP], F32) for i in range(2)]
        GBC = [sb("GBC%d" % i, [128, TP], F32) for i in range(2)]
        WR = sb("WR", [128, DC, 36], F32)
        BR = sb("BR", [128, 36], F32)
        IDENT = sb("IDENT", [128, 128], F32)
        ONES = sb("ONES", [128, 128], F32)
        LNP = sb("LNP", [128, 4, DC], F32)
        GT = sb("GT", [32, TP], F32)
        MEAN = sb("MEAN", [128, TP], F32)
        RSTD = sb("RSTD", [128, TP], F32)
        SQ = [sb("SQ%d" % i, [128, TP], F32) for i in range(2)]
        OUT = [sb("OUT%d" % i, [128, TP], F32) for i in range(2)]
        LG4 = sb("LG4", [128, 4, 36], F32)
        R = {n: sb("r_" + n, [128, 32], F32) for n in ("em", "em2", "m1", "m2", "g1", "gates", "gexp")}
        C = {n: sb("c_" + n, [128, 4], F32) for n in
             ("gmax", "ngmax", "gsum", "gw", "goh", "pen", "v1", "v2", "d", "ed", "den", "w1", "w2")}
        PS = [es.enter_context(nc.psum_tensor("ps%d" % i, [128, 512], F32)) for i in range(8)]
        es.enter_context(nc.Block())

        P = Prog(nc)
        bACC = [Buf("acc%d" % c) for c in range(DC)]
        bXB = Buf("xb")
        bHT = [Buf(), Buf()]
        bS = [Buf(), Buf(), Buf(), Buf()]
        bTT = [Buf(), Buf()]
        bGBC = [Buf(), Buf()]
        bPS = [Buf("ps%d" % i) for i in range(8)]
        bC = Buf("consts")
        bR = Buf("router")
        bGT = Buf("gt")
        bLG4 = Buf("lg4")
        bMEAN = Buf("mean")
        bRSTD = Buf("rstd")
        bSQ = [Buf(), Buf()]
        bOUT = [Buf(), Buf()]
        ring = Ring(P, RINGT, [Buf("ring%d" % i) for i in range(NSLOT)])
        PH, PY, PGB, PMISC = (0, 1, 2, 3), (4, 5), 6, 7
        V, A, PE, G = nc.vector, nc.scalar, nc.tensor, nc.gpsimd

        P.dma("sync", "c", WR[:], wr.rearrange("(c p) n -> p c n", p=128), writes=[bC])
        P.dma("sync", "c", BR[:], br, writes=[bC])
        P.dma("sync", "c", IDENT[:], ident_d, writes=[bC])
        P.dma("sync", "c", LNP[:], lnp, writes=[bC])
        P.op("vector", lambda: V.memset(ONES[:], 1.0 / D), writes=[bC])

        def ld_cols(src2d, col0):
            def f(tile, buf):
                srcv = src2d.rearrange("(c p) f -> p c f", p=128)
                dst = tile[:].rearrange("p (c f) -> p c f", f=128)
                for q in range(2):
                    P.dma("gpsimd", "w", dst[:, q * 16:(q + 1) * 16, :],
                          srcv[:, q * 16:(q + 1) * 16, col0:col0 + 128], writes=[buf])
            return f

        def ld_krange(src2d, kp, col0=0):
            def f(tile, buf):
                srcv = src2d.rearrange("(c p) f -> p c f", p=128)
                dst = tile[:].rearrange("p (c f) -> p c f", f=512)
                P.dma("gpsimd", "w", dst, srcv[:, kp * 8:(kp + 1) * 8, col0:col0 + 512], writes=[buf])
            return f

        def ld_w2(src2d, g):
            def f(tile, buf):
                srcv = src2d.rearrange("(j p) d -> p j d", p=128)
                dst = tile[:].rearrange("p (j d) -> p j d", d=1024)
                P.dma("gpsimd", "w", dst, srcv[:, :, g * 1024:(g + 1) * 1024], writes=[buf])
            return f

        npass = T // TP
        for ps_i in range(npass):
            if with_proj:
                for dcg in range(DC // 4):
                    for kp in range(4):
                        ring.push(ld_krange(wout, kp, dcg * 512))
            for e in range(n_exp):
                for kp in range(4):
                    ring.push(ld_krange(w1[e], kp))
                for kp in range(4):
                    ring.push(ld_krange(w3[e], kp))
                for g in range(4):
                    ring.push(ld_w2(w2[e], g))

        def layer_norm(kg, kb, store_t0=None):
            pm, pq = PY[0], PY[1]
            for c in range(DC):
                P.op("tensor", (lambda c=c: PE.matmul(PS[pm][:], lhsT=ONES[:], rhs=ACC[:, c, :],
                                                       start=(c == 0), stop=(c == DC - 1))),
                     reads=[bC, bACC[c]], writes=[bPS[pm]])
            for c in range(DC):
                qb = c % 2
                P.op("scalar", (lambda c=c, qb=qb: A.activation(out=SQ[qb][:], in_=ACC[:, c, :], func=AF.Square)),
                     reads=[bACC[c]], writes=[bSQ[qb]])
                P.op("tensor", (lambda c=c, qb=qb: PE.matmul(PS[pq][:], lhsT=ONES[:], rhs=SQ[qb][:],
                                                              start=(c == 0), stop=(c == DC - 1))),
                     reads=[bC, bSQ[qb]], writes=[bPS[pq]])
            P.op("vector", lambda: V.tensor_copy(out=MEAN[:], in_=PS[pm][:]), reads=[bPS[pm]], writes=[bMEAN])
            P.op("vector", lambda: V.tensor_tensor(out=RSTD[:], in0=MEAN[:], in1=MEAN[:], op=ALU.mult),
                 reads=[bMEAN], writes=[bRSTD])
            P.op("vector", lambda: V.tensor_tensor(out=RSTD[:], in0=PS[pq][:], in1=RSTD[:], op=ALU.subtract),
                 reads=[bPS[pq], bRSTD], writes=[bRSTD])
            P.op("vector", lambda: V.tensor_scalar(out=RSTD[:], in0=RSTD[:], scalar1=EPS, scalar2=None, op0=ALU.add),
                 reads=[bRSTD], writes=[bRSTD])
            P.op("scalar", lambda: A.activation(out=RSTD[:], in_=RSTD[:], func=AF.Sqrt), reads=[bRSTD], writes=[bRSTD])
            P.op("vector", lambda: V.reciprocal(out=RSTD[:], in_=RSTD[:]), reads=[bRSTD], writes=[bRSTD])
            ov = xout.rearrange("(c p) t -> p c t", p=128)
            for c in range(DC):
                P.op("vector", (lambda c=c: V.tensor_tensor(out=ACC[:, c, :], in0=ACC[:, c, :], in1=MEAN[:], op=ALU.subtract)),
                     reads=[bMEAN, bACC[c]], writes=[bACC[c]])
                P.op("vector", (lambda c=c: V.tensor_tensor(out=ACC[:, c, :], in0=ACC[:, c, :], in1=RSTD[:], op=ALU.mult)),
                     reads=[bRSTD, bACC[c]], writes=[bACC[c]])
                if store_t0 is None:
                    P.op("scalar", (lambda c=c: A.activation(out=ACC[:, c, :], in_=ACC[:, c, :], func=AF.Identity,
                                                              bias=LNP[:, kb, c:c + 1], scale=LNP[:, kg, c:c + 1])),
                         reads=[bACC[c], bC], writes=[bACC[c]])
                else:
                    ob = c % 2
                    P.op("scalar", (lambda c=c, ob=ob: A.activation(out=OUT[ob][:], in_=ACC[:, c, :], func=AF.Identity,
                                                                     bias=LNP[:, kb, c:c + 1], scale=LNP[:, kg, c:c + 1])),
                         reads=[bACC[c], bC], writes=[bOUT[ob]])
                    P.dma("sync", "out", ov[:, c, store_t0:store_t0 + TP], OUT[ob][:], reads=[bOUT[ob]])

        def scale_acc():
            for c in range(DC):
                P.op("gpsimd", (lambda c=c: G.tensor_scalar(out=ACC[:, c, :], in0=ACC[:, c, :], scalar1=ALPHA,
                                                             scalar2=0.0, op0=ALU.mult, op1=ALU.add)),
                     reads=[bACC[c]], writes=[bACC[c]])

        def router_a():
            for s_ in range(TP // 128):
                ts = slice(s_ * 128, (s_ + 1) * 128)
                for c in range(DC):
                    P.op("tensor", (lambda c=c, ts=ts, s_=s_: PE.matmul(PS[PMISC][:, s_ * 36:(s_ + 1) * 36], lhsT=ACC[:, c, ts], rhs=WR[:, c, :],
                                                                          start=(c == 0), stop=(c == DC - 1))),
                         reads=[bACC[c], bC], writes=[bPS[PMISC]])
            for s_ in range(TP // 128):
                P.op("vector", (lambda s_=s_: V.tensor_tensor(out=LG4[:, s_, :], in0=PS[PMISC][:, s_ * 36:(s_ + 1) * 36], in1=BR[:], op=ALU.add)),
                     reads=[bPS[PMISC], bC], writes=[bLG4])

        def router_b():
            rw = [bR]
            for s_ in range(TP // 128):
                ts = slice(s_ * 128, (s_ + 1) * 128)
                LG = LG4[:, s_, :]

                def v(fn, extra_reads=()):
                    P.op("vector", fn, reads=rw + list(extra_reads), writes=rw)

                def a(fn):
                    P.op("scalar", fn, reads=rw, writes=rw)

                v(lambda LG=LG: V.reduce_max(out=C["gmax"][:, 0:1], in_=LG[:, 0:4], axis=AX.X), [bLG4])
                v(lambda LG=LG: V.tensor_scalar(out=C["goh"][:, 0:4], in0=LG[:, 0:4], scalar1=C["gmax"][:, 0:1],
                                                scalar2=None, op0=ALU.is_ge))
                v(lambda: V.tensor_scalar(out=C["ngmax"][:, 0:1], in0=C["gmax"][:, 0:1], scalar1=-1.0,
                                          scalar2=None, op0=ALU.mult))
                a(lambda LG=LG: A.activation(out=R["gexp"][:, 0:4], in_=LG[:, 0:4], func=AF.Exp,
                                             bias=C["ngmax"][:, 0:1], scale=1.0, accum_out=C["gsum"][:, 0:1]))
                v(lambda: V.reciprocal(out=C["gw"][:, 0:1], in_=C["gsum"][:, 0:1]))
                v(lambda: V.tensor_scalar(out=C["pen"][:, 0:4], in0=C["goh"][:, 0:4], scalar1=-1.0,
                                          scalar2=BIG, op0=ALU.add, op1=ALU.mult))
                for g in range(4):
                    v(lambda g=g, LG=LG: V.tensor_scalar(out=R["em"][:, g * 8:(g + 1) * 8],
                                                         in0=LG[:, 4 + g * 8:4 + (g + 1) * 8],
                                                         scalar1=C["pen"][:, g:g + 1], scalar2=None, op0=ALU.add))
                v(lambda: V.reduce_max(out=C["v1"][:, 0:1], in_=R["em"][:], axis=AX.X))
                v(lambda: V.tensor_scalar(out=R["m1"][:], in0=R["em"][:], scalar1=C["v1"][:, 0:1],
                                          scalar2=None, op0=ALU.is_ge))
                v(lambda: V.scalar_tensor_tensor(out=R["em2"][:], in0=R["m1"][:], scalar=-BIG, in1=R["em"][:],
                                                 op0=ALU.mult, op1=ALU.add))
                v(lambda: V.reduce_max(out=C["v2"][:, 0:1], in_=R["em2"][:], axis=AX.X))
                v(lambda: V.tensor_scalar(out=R["m2"][:], in0=R["em2"][:], scalar1=C["v2"][:, 0:1],
                                          scalar2=None, op0=ALU.is_ge))
                v(lambda: V.tensor_tensor(out=C["d"][:, 0:1], in0=C["v2"][:, 0:1], in1=C["v1"][:, 0:1],
                                          op=ALU.subtract))
                a(lambda: A.activation(out=C["ed"][:, 0:1], in_=C["d"][:, 0:1], func=AF.Exp))
                v(lambda: V.tensor_scalar(out=C["den"][:, 0:1], in0=C["ed"][:, 0:1], scalar1=1.0,
                                          scalar2=None, op0=ALU.add))
                v(lambda: V.reciprocal(out=C["w1"][:, 0:1], in_=C["den"][:, 0:1]))
                v(lambda: V.tensor_tensor(out=C["w2"][:, 0:1], in0=C["ed"][:, 0:1], in1=C["w1"][:, 0:1], op=ALU.mult))
                v(lambda: V.tensor_tensor(out=C["w1"][:, 0:1], in0=C["w1"][:, 0:1], in1=C["gw"][:, 0:1], op=ALU.mult))
                v(lambda: V.tensor_tensor(out=C["w2"][:, 0:1], in0=C["w2"][:, 0:1], in1=C["gw"][:, 0:1], op=ALU.mult))
                v(lambda: V.tensor_scalar(out=R["g1"][:], in0=R["m1"][:], scalar1=C["w1"][:, 0:1],
                                          scalar2=None, op0=ALU.mult))
                v(lambda: V.scalar_tensor_tensor(out=R["gates"][:], in0=R["m2"][:], scalar=C["w2"][:, 0:1],
                                                 in1=R["g1"][:], op0=ALU.mult, op1=ALU.add))
                P.op("tensor", lambda: PE.transpose(PS[PMISC][0:32, 256:384], R["gates"][:], IDENT[:]),
                     reads=[bR, bC], writes=[bPS[PMISC]])
                P.op("vector", (lambda ts=ts: V.tensor_copy(out=GT[:, ts], in_=PS[PMISC][0:32, 256:384])),
                     reads=[bPS[PMISC]], writes=[bGT])

        def cast_acc_to_xb():
            for c in range(DC):
                if c % 2 == 0:
                    P.op("vector", (lambda c=c: V.tensor_copy(out=XB[:, c, :], in_=ACC[:, c, :])),
                         reads=[bACC[c]], writes=[bXB])
                else:
                    P.op("scalar", (lambda c=c: A.copy(out=XB[:, c, :], in_=ACC[:, c, :])),
                         reads=[bACC[c]], writes=[bXB])

        def experts():
            def gate_bcast(e, hb):
                P.op("tensor", (lambda e=e: PE.matmul(PS[PGB][:], lhsT=IDENT[0:32, e:e + 1].to_broadcast([32, 128]), rhs=GT[:],
                                                       start=True, stop=True)),
                     reads=[bC, bGT], writes=[bPS[PGB]])
                P.op("scalar", (lambda hb=hb: A.copy(out=GBC[hb][:], in_=PS[PGB][:])),
                     reads=[bPS[PGB]], writes=[bGBC[hb]])

            for e in range(n_exp):
                hb = e % 2
                if e > 0:
                    gate_bcast(e, hb)
                for (kind, SRC) in ((0, None), (1, None)):
                    for kp in range(4):
                        tw, bw = ring.take()
                        Wv = tw[:].rearrange("p (c f) -> p c f", f=512)
                        for j in range(4):
                            for c in range(8):
                                P.op("tensor", (lambda c=c, j=j, kp=kp, Wv=Wv: PE.matmul(PS[PH[j]][:], lhsT=Wv[:, c, j * 128:(j + 1) * 128],
                                                                                         rhs=XB[:, kp * 8 + c, :],
                                                                                         start=(kp == 0 and c == 0), stop=(kp == 3 and c == 7))),
                                     reads=[bw, bXB], writes=[bPS[PH[j]]])
                        ring.release(1)
                    if e == 0 and kind == 1:
                        router_b()
                        gate_bcast(e, hb)
                    for j in range(4):
                        if kind == 0:
                            P.op("scalar", (lambda j=j: A.activation(out=S[j][:], in_=PS[PH[j]][:], func=AF.Silu)),
                                 reads=[bPS[PH[j]]], writes=[bS[j]])
                        else:
                            jb = j % 2
                            P.op("vector", (lambda j=j, jb=jb: V.tensor_tensor(out=TT[jb][:], in0=PS[PH[j]][:], in1=S[j][:], op=ALU.mult)),
                                 reads=[bPS[PH[j]], bS[j]], writes=[bTT[jb]])
                            P.op("vector", (lambda jb=jb, hb=hb, j=j: V.tensor_tensor(out=HT[hb][:, j, :], in0=TT[jb][:],
                                                                                       in1=GBC[hb][:], op=ALU.mult)),
                                 reads=[bTT[jb], bGBC[hb]], writes=[bHT[hb]])
                for g in range(4):
                    t2, b2 = ring.take()
                    W2v = t2[:].rearrange("p (j d) -> p j d", d=1024)
                    for dl in range(8):
                        dc = g * 8 + dl
                        py = PY[dc % 2]
                        for j in range(4):
                            P.op("tensor", (lambda j=j, dl=dl, W2v=W2v, py=py, hb=hb:
                                            PE.matmul(PS[py][:], lhsT=W2v[:, j, dl * 128:(dl + 1) * 128],
                                                      rhs=HT[hb][:, j, :], start=(j == 0), stop=(j == 3))),
                                 reads=[b2, bHT[hb]], writes=[bPS[py]])
                        P.op("vector", (lambda dc=dc, py=py: V.tensor_tensor(out=ACC[:, dc, :], in0=PS[py][:],
                                                                              in1=ACC[:, dc, :], op=ALU.add)),
                             reads=[bPS[py], bACC[dc]], writes=[bACC[dc]])
                    ring.release()

        for ps_i in range(npass):
            t0 = ps_i * TP
            xv = xres.rearrange("(c p) t -> p c t", p=128)
            for q in range(4):
                P.dma("sync", "in", ACC[:, q * 8:(q + 1) * 8, :], xv[:, q * 8:(q + 1) * 8, t0:t0 + TP],
                      writes=bACC[q * 8:(q + 1) * 8])
            if with_proj:
                mv = mix.rearrange("(c p) t -> p c t", p=128)
                for q in range(4):
                    P.dma("gpsimd", "w", XB[:, q * 8:(q + 1) * 8, :], mv[:, q * 8:(q + 1) * 8, t0:t0 + TP],
                          writes=[bXB])
                scale_acc()
                for dcg in range(DC // 4):
                    for kp in range(4):
                        tw, bw = ring.take()
                        Wv = tw[:].rearrange("p (c f) -> p c f", f=512)
                        for j in range(4):
                            for c in range(8):
                                P.op("tensor", (lambda c=c, j=j, kp=kp, Wv=Wv: PE.matmul(PS[PH[j]][:], lhsT=Wv[:, c, j * 128:(j + 1) * 128],
                                                                                         rhs=XB[:, kp * 8 + c, :],
                                                                                         start=(kp == 0 and c == 0), stop=(kp == 3 and c == 7))),
                                     reads=[bw, bXB], writes=[bPS[PH[j]]])
                        ring.release(1)
                    for j in range(4):
                        dc = dcg * 4 + j
                        P.op("vector", (lambda dc=dc, j=j: V.tensor_tensor(out=ACC[:, dc, :], in0=PS[PH[j]][:],
                                                                            in1=ACC[:, dc, :], op=ALU.add)),
                             reads=[bPS[PH[j]], bACC[dc]], writes=[bACC[dc]])
                layer_norm(0, 1)
            cast_acc_to_xb()
            router_a()
            scale_acc()
            experts()
            layer_norm(2, 3, store_t0=t0)
        stats = P.emit(es)
    return nc, stats


def tail_consts():
    sel = np.zeros((32, NE * 128), np.float32)
    for e in range(NE):
        sel[e, e * 128:(e + 1) * 128] = 1.0
    return dict(ident=np.eye(128, dtype=np.float32), sel=sel)


def pc(v):
    return np.ascontiguousarray(np.asarray(v, np.float32).reshape(DC, 128).T)


def build_mix0(S=SEQ, n_units=5, stage=3):
    nc = bass.Bass("TRN2", target_bir_lowering=False)
    dt = nc.dram_tensor
    NT = S // TP
    NB = S // 128
    xT = dt("xT", [D, S], F32, kind="ExternalInput").ap()
    wp = dt("wp", [D, 512], F32, kind="ExternalInput").ap()
    wqkv = dt("wqkv", [4, D, 384], F32, kind="ExternalInput").ap()
    wf = dt("wf", [128, DC * 4], F32, kind="ExternalInput").ap()
    smallc = dt("smallc", [128, 64], F32, kind="ExternalInput").ap()
    wpool = dt("wpool", [512, 512], F32, kind="ExternalInput").ap()
    masks = dt("masks", [4, 128, 512], F32, kind="ExternalInput").ap()
    tri_d = dt("tri", [128, 128], F32, kind="ExternalInput").ap()
    ident_d = dt("ident", [128, 128], F32, kind="ExternalInput").ap()
    aT = dt("aT", [512, S], F32, kind="ExternalOutput").ap()
    bT = dt("bT", [512, S], F32, kind="ExternalOutput").ap()

    es = contextlib.ExitStack()
    with es:
        def sb(name, shape, dtype):
            return es.enter_context(nc.sbuf_tensor(name, shape, dtype))

        XT = [sb("XT%d" % i, [128, DC, TP], BF16) for i in range(2)]
        WU = [sb("WU%d" % i, [128, DC, 512], BF16) for i in range(2)]
        WF = sb("WF", [128, DC, 4], BF16)
        SMALL = sb("SMALL", [128, 64], F32)
        BFR = SMALL[:, 0:4]
        PSC = SMALL[:, 4:8]
        PCO = SMALL[:, 8:12]
        PCR = SMALL[:, 12:28]
        WPOOL = sb("WPOOL", [128, 4, 512], BF16)
        MASK = sb("MASK", [128, 4, 512], BF16)
        TRI = sb("TRI", [128, 128], F32)
        IDENT = sb("IDENT", [128, 128], F32)
        ONESF = sb("ONESF", [128, 128], F32)
        ONESB = sb("ONESB", [128, 128], BF16)
        U = sb("U", [128, 4, 16 + TP], F32)
        S2 = sb("S2", [128, 16 + TP], F32)
        S4 = sb("S4", [128, 16 + TP], F32)
        S8 = sb("S8", [128, 16 + TP], F32)
        S16 = sb("S16", [128, 16 + TP], F32)
        PA = sb("PA", [128, TP], F32)
        PLB = sb("PLB", [128, 4, TP], BF16)
        AO = [sb("AO%d" % i, [128, TP], F32) for i in range(2)]
        QT = sb("QT", [128, S], BF16)
        KT = sb("KT", [128, S], BF16)
        VV = sb("VV", [128, NB, 128], BF16)
        SP = sb("SP", [128, NB], F32)
        SPX = sb("SPX", [128, NB], F32)
        CN = sb("CN", [128, NB], F32)
        FZ = sb("FZ", [128, 4], F32)
        NMQ = sb("NMQ", [1, S], BF16)
        PT = [sb("PT%d" % i, [128, TP], BF16) for i in range(2)]
        RS = sb("RS", [128, TP], F32)
        EXA = [S2[:, 0:TP], S4[:, 0:TP]]
        BO = [sb("BO%d" % i, [128, TP], F32) for i in range(2)]
        PS = [es.enter_context(nc.psum_tensor("ps%d" % i, [128, 512], F32)) for i in range(8)]
        es.enter_context(nc.Block())

        P = Prog(nc)
        V, A, PE, G = nc.vector, nc.scalar, nc.tensor, nc.gpsimd
        bXT = [Buf(), Buf()]
        bWU = [Buf(), Buf()]
        bC = Buf("consts")
        bU = Buf("u")
        bS = Buf("swork")
        bPA = Buf("pa")
        bPLB = Buf("plb")
        bAO = [Buf(), Buf()]
        bQT, bKT, bVV, bSP, bCN, bNMQ = Buf(), Buf(), Buf(), Buf(), Buf(), Buf()
        bPT = [Buf(), Buf()]
        bEXA = [Buf(), Buf()]
        bRS = Buf()
        bBO = [Buf(), Buf()]
        bPS = [Buf("ps%d" % i) for i in range(8)]
        PPROJ, PSC_, POT, PSM, PMISC = (0, 1), (2, 3), 4, 5, (6, 7)

        P.dma("gpsimd", "c", WF[:].rearrange("p c h -> p (c h)"), wf, writes=[bC])
        P.dma("sync", "c", SMALL[:], smallc, writes=[bC])
        P.dma("gpsimd", "c", WPOOL[:], wpool.rearrange("(c p) d -> p c d", p=128), writes=[bC])
        P.dma("gpsimd", "c", MASK[:], masks.rearrange("m p q -> p m q"), writes=[bC])
        P.dma("sync", "c", TRI[:], tri_d, writes=[bC])
        P.dma("sync", "c", IDENT[:], ident_d, writes=[bC])
        P.op("vector", lambda: V.memset(ONESF[:], 1.0), writes=[bC])
        P.op("vector", lambda: V.memset(ONESB[:], 1.0), writes=[bC])
        P.op("vector", lambda: V.memset(U[:], 0.0), writes=[bU])

        xv = xT.rearrange("(c p) t -> p c t", p=128)

        def load_x(i, slot):
            for q in range(4):
                P.dma("gpsimd", "x", XT[slot][:, q * 8:(q + 1) * 8, :], xv[:, q * 8:(q + 1) * 8, i * TP:(i + 1) * TP],
                      writes=[bXT[slot]])

        def load_w(unit, slot):
            if unit == 0:
                srcv = wp.rearrange("(c p) f -> p c f", p=128)
                for q in range(4):
                    P.dma("gpsimd", "x", WU[slot][:, q * 8:(q + 1) * 8, :], srcv[:, q * 8:(q + 1) * 8, :], writes=[bWU[slot]])
            else:
                srcv = wqkv[unit - 1].rearrange("(c p) f -> p c f", p=128)
                for q in range(4):
                    P.dma("gpsimd", "x", WU[slot][:, q * 8:(q + 1) * 8, 0:384], srcv[:, q * 8:(q + 1) * 8, :],
                          writes=[bWU[slot]])

        load_w(0, 0)
        load_x(0, 0)
        load_x(1, 1)
        xcount = 0

        for unit in range(n_units):
            ws = unit % 2
            W = WU[ws]
            for i in range(NT):
                xs_ = xcount % 2
                X = XT[xs_]
                if unit == 0:
                    for c4 in range(4):
                        pp = PPROJ[c4 % 2]
                        for c in range(DC):
                            P.op("tensor", (lambda c=c, c4=c4, pp=pp, X=X, W=W: PE.matmul(PS[pp][:], lhsT=W[:, c, c4 * 128:(c4 + 1) * 128],
                                                                                       rhs=X[:, c, :], start=(c == 0), stop=(c == DC - 1))),
                                 reads=[bWU[ws], bXT[xs_]], writes=[bPS[pp]])
                        P.op("scalar", (lambda c4=c4, pp=pp: A.copy(out=U[:, c4, 16:16 + TP], in_=PS[pp][:])),
                             reads=[bPS[pp]], writes=[bU])
                    for c4 in range(4):
                        u = U[:, c4, :]
                        L = 16 + TP
                        P.op("vector", (lambda u=u: V.tensor_tensor(out=S2[:, 1:L], in0=u[:, 1:L], in1=u[:, 0:L - 1], op=ALU.add)),
                             reads=[bU, bS], writes=[bS])
                        P.op("vector", lambda: V.tensor_tensor(out=S4[:, 3:L], in0=S2[:, 3:L], in1=S2[:, 1:L - 2], op=ALU.add),
                             reads=[bS], writes=[bS])
                        P.op("vector", lambda: V.tensor_tensor(out=S8[:, 7:L], in0=S4[:, 7:L], in1=S4[:, 3:L - 4], op=ALU.add),
                             reads=[bS], writes=[bS])
                        P.op("vector", lambda: V.tensor_tensor(out=S16[:, 15:L], in0=S8[:, 15:L], in1=S8[:, 7:L - 8], op=ALU.add),
                             reads=[bS], writes=[bS])
                        P.op("vector", lambda: V.tensor_scalar(out=PA[:], in0=S2[:, 16:L], scalar1=PCO[:, 0:1], scalar2=None, op0=ALU.mult),
                             reads=[bS, bC, bPA], writes=[bPA])
                        for wi, SW in ((1, S4), (2, S8), (3, S16)):
                            P.op("vector", (lambda wi=wi, SW=SW: V.scalar_tensor_tensor(out=PA[:], in0=SW[:, 16:L], scalar=PCO[:, wi:wi + 1],
                                                                                         in1=PA[:], op0=ALU.mult, op1=ALU.add)),
                                 reads=[bS, bC, bPA], writes=[bPA])
                        if i == 0:
                            P.op("vector", lambda: V.tensor_tensor(out=PA[:, 0:16], in0=PA[:, 0:16], in1=PCR, op=ALU.mult),
                                 reads=[bPA, bC], writes=[bPA])
                        P.op("vector", (lambda c4=c4, u=u: V.tensor_tensor(out=PLB[:, c4, :], in0=PA[:], in1=u[:, 16:L], op=ALU.subtract)),
                             reads=[bPA, bU], writes=[bPLB])
                        P.op("vector", (lambda u=u: V.tensor_copy(out=u[:, 0:16], in_=u[:, TP:TP + 16])),
                             reads=[bU, bPLB], writes=[bU])
                    for o4 in range(4):
                        pp = PSC_[o4 % 2]
                        for c4 in range(4):
                            P.op("tensor", (lambda c4=c4, o4=o4, pp=pp: PE.matmul(PS[pp][:], lhsT=WPOOL[:, c4, o4 * 128:(o4 + 1) * 128],
                                                                                    rhs=PLB[:, c4, :], start=(c4 == 0), stop=(c4 == 3))),
                                 reads=[bC, bPLB], writes=[bPS[pp]])
                        ab = o4 % 2
                        P.op("scalar", (lambda o4=o4, pp=pp, ab=ab: A.activation(out=AO[ab][:], in_=PS[pp][:], func=AF.Identity,
                                                                                  bias=0.0, scale=PSC[:, o4:o4 + 1])),
                             reads=[bPS[pp], bC], writes=[bAO[ab]])
                        P.dma("sync", "out", aT[o4 * 128:(o4 + 1) * 128, i * TP:(i + 1) * TP], AO[ab][:], reads=[bAO[ab]])
                else:
                    h = unit - 1
                    for kind, dst, bdst in ((0, QT, bQT), (1, KT, bKT)):
                        pp = PPROJ[kind]
                        for c in range(DC):
                            P.op("tensor", (lambda c=c, kind=kind, pp=pp, X=X, W=W: PE.matmul(PS[pp][:], lhsT=W[:, c, kind * 128:(kind + 1) * 128],
                                                                                           rhs=X[:, c, :], start=(c == 0), stop=(c == DC - 1))),
                                 reads=[bWU[ws], bXT[xs_]], writes=[bPS[pp]])
                        sc = HEAD_DIM ** -0.5 if kind == 0 else 1.0
                        P.op("scalar", (lambda dst=dst, pp=pp, sc=sc, i=i: A.activation(out=dst[:, i * TP:(i + 1) * TP], in_=PS[pp][:],
                                                                                         func=AF.Identity, bias=0.0, scale=sc)),
                             reads=[bPS[pp]], writes=[bdst])
                    pv = PSC_[0]
                    for tb in range(4):
                        for c in range(DC):
                            P.op("tensor", (lambda c=c, tb=tb, X=X, W=W: PE.matmul(PS[pv][:, tb * 128:(tb + 1) * 128], lhsT=X[:, c, tb * 128:(tb + 1) * 128],
                                                                               rhs=W[:, c, 256:384], start=(c == 0), stop=(c == DC - 1))),
                                 reads=[bWU[ws], bXT[xs_]], writes=[bPS[pv]])
                    P.op("vector", (lambda i=i: V.tensor_copy(out=VV[:, i * 4:(i + 1) * 4, :].rearrange("p b d -> p (b d)"), in_=PS[pv][:])),
                         reads=[bPS[pv]], writes=[bVV])
                    pf = PSC_[1]
                    for tb in range(4):
                        for c in range(DC):
                            P.op("tensor", (lambda c=c, tb=tb, X=X, h=h: PE.matmul(PS[pf][:, tb:tb + 1], lhsT=X[:, c, tb * 128:(tb + 1) * 128],
                                                                                    rhs=WF[:, c, h:h + 1], start=(c == 0), stop=(c == DC - 1))),
                                 reads=[bC, bXT[xs_]], writes=[bPS[pf]])
                    P.op("vector", (lambda h=h: V.tensor_scalar(out=FZ[:], in0=PS[pf][:, 0:4], scalar1=BFR[:, h:h + 1], scalar2=-60.0,
                                                                 op0=ALU.add, op1=ALU.max)),
                         reads=[bPS[pf], bC, bSP], writes=[bSP])
                    P.op("scalar", lambda: A.activation(out=FZ[:], in_=FZ[:], func=AF.Exp, scale=-1.0), reads=[bSP], writes=[bSP])
                    P.op("scalar", (lambda i=i: A.activation(out=SP[:, i * 4:(i + 1) * 4], in_=FZ[:], func=AF.Ln, bias=1.0, scale=1.0)),
                         reads=[bSP], writes=[bSP])
                xcount += 1
                nxt = xcount + 1
                if nxt < n_units * NT:
                    load_x(nxt % NT, nxt % 2)
                if i == 0 and unit + 1 < n_units:
                    load_w(unit + 1, (unit + 1) % 2)
            if unit == 0 or stage < 2:
                continue
            h = unit - 1
            P.op("vector", lambda: V.memset(SPX[:, 0:1], 0.0), reads=[bSP], writes=[bSP])
            P.op("vector", lambda: V.tensor_tensor_scan(out=SPX[:, 1:NB], data0=ONESF[:, 0:NB - 1], data1=SP[:, 0:NB - 1], initial=0.0,
                                                        op0=ALU.mult, op1=ALU.add), reads=[bSP, bC], writes=[bSP])
            pm = PMISC[0]
            P.op("tensor", lambda: PE.matmul(PS[pm][:, 0:NB], lhsT=TRI[:], rhs=SP[:], start=True, stop=False),
                 reads=[bC, bSP], writes=[bPS[pm]])
            P.op("tensor", lambda: PE.matmul(PS[pm][:, 0:NB], lhsT=ONESF[:], rhs=SPX[:], start=False, stop=True),
                 reads=[bC, bSP], writes=[bPS[pm]])
            P.op("vector", lambda: V.tensor_copy(out=CN[:], in_=PS[pm][:, 0:NB]), reads=[bPS[pm]], writes=[bCN])
            pr = PMISC[1]
            for g in range(NT):
                for tb in range(4):
                    kb = g * 4 + tb
                    P.op("tensor", (lambda kb=kb, tb=tb: PE.matmul(PS[pr][0:1, tb * 128:(tb + 1) * 128], lhsT=CN[:, kb:kb + 1], rhs=IDENT[:],
                                                                    start=True, stop=True)),
                         reads=[bCN, bC], writes=[bPS[pr]])
                P.op("scalar", (lambda g=g: A.activation(out=NMQ[0:1, g * TP:(g + 1) * TP], in_=PS[pr][0:1, :], func=AF.Identity,
                                                          bias=0.0, scale=-1.0)),
                     reads=[bPS[pr]], writes=[bNMQ])
            if stage < 3:
                continue
            items = [(g, kk) for g in range(NT) for kk in range(4 * g + 4)]

            def emit_scores(idx):
                g, kk = items[idx]
                psb = PSC_[idx % 2]
                P.op("tensor", (lambda kk=kk, g=g, psb=psb: PE.matmul(PS[psb][:], lhsT=KT[:, kk * 128:(kk + 1) * 128], rhs=QT[:, g * TP:(g + 1) * TP],
                                                                       start=True, stop=False)),
                     reads=[bKT, bQT], writes=[bPS[psb]])
                P.op("tensor", (lambda g=g, psb=psb: PE.matmul(PS[psb][:], lhsT=ONESB[0:1, :], rhs=NMQ[0:1, g * TP:(g + 1) * TP],
                                                                start=False, stop=True)),
                     reads=[bC, bNMQ], writes=[bPS[psb]])

            emit_scores(0)
            for idx, (g, kk) in enumerate(items):
                nk = 4 * g + 4
                psb = PSC_[idx % 2]
                pb = idx % 2
                if kk >= 4 * g:
                    P.op("vector", (lambda kk=kk, psb=psb, pb=pb: V.tensor_scalar(out=EXA[pb], in0=PS[psb][:], scalar1=CN[:, kk:kk + 1],
                                                                                   scalar2=60.0, op0=ALU.add, op1=ALU.min)),
                         reads=[bPS[psb], bCN], writes=[bEXA[pb]])
                    P.op("scalar", (lambda pb=pb: A.activation(out=PT[pb][:], in_=EXA[pb], func=AF.Exp)),
                         reads=[bEXA[pb]], writes=[bPT[pb]])
                    P.op("vector", (lambda kk=kk, g=g, pb=pb: V.tensor_tensor(out=PT[pb][:], in0=PT[pb][:], in1=MASK[:, kk - 4 * g, :], op=ALU.mult)),
                         reads=[bPT[pb], bC], writes=[bPT[pb]])
                else:
                    P.op("scalar", (lambda kk=kk, psb=psb, pb=pb: A.activation(out=PT[pb][:], in_=PS[psb][:], func=AF.Exp,
                                                                                bias=CN[:, kk:kk + 1], scale=1.0)),
                         reads=[bPS[psb], bCN], writes=[bPT[pb]])
                if idx + 1 < len(items):
                    emit_scores(idx + 1)
                P.op("tensor", (lambda kk=kk, pb=pb, nk=nk: PE.matmul(PS[POT][:], lhsT=VV[:, kk, :], rhs=PT[pb][:], start=(kk == 0), stop=(kk == nk - 1))),
                     reads=[bVV, bPT[pb]], writes=[bPS[POT]])
                P.op("tensor", (lambda kk=kk, pb=pb, nk=nk: PE.matmul(PS[PSM][:], lhsT=ONESB[:], rhs=PT[pb][:], start=(kk == 0), stop=(kk == nk - 1))),
                     reads=[bC, bPT[pb]], writes=[bPS[PSM]])
                if kk == nk - 1:
                    P.op("vector", lambda: V.reciprocal(out=RS[:], in_=PS[PSM][:]), reads=[bPS[PSM], bRS], writes=[bRS])
                    ob = g % 2
                    P.op("vector", (lambda ob=ob: V.tensor_tensor(out=BO[ob][:], in0=PS[POT][:], in1=RS[:], op=ALU.mult)),
                         reads=[bPS[POT], bRS], writes=[bBO[ob]])
                    P.dma("sync", "out", bT[h * 128:(h + 1) * 128, g * TP:(g + 1) * TP], BO[ob][:], reads=[bBO[ob]])
        stats = P.emit(es)
    return nc, stats


def mix0_consts():
    masks = np.zeros((4, 128, 512), np.float32)
    kl = np.arange(128)[:, None]
    ql = np.arange(512)[None, :]
    for m in range(4):
        masks[m] = (128 * m + kl <= ql).astype(np.float32)
    s = np.arange(128)
    tri = (s[:, None] <= s[None, :]).astype(np.float32)
    return dict(masks=masks, tri=tri, ident=np.eye(128, dtype=np.float32))


GELU_C = 0.7978845608028654


def build_mix1(S=SEQ, n_units=4):
    nc = bass.Bass("TRN2", target_bir_lowering=False)
    dt = nc.dram_tensor
    NT = S // TP
    xT = dt("xT", [D, S], F32, kind="ExternalInput").ap()
    win = dt("win", [4, D, 512], F32, kind="ExternalInput").ap()
    wa = dt("wa", [4, 256, 256], F32, kind="ExternalInput").ap()
    wx = dt("wx", [4, 256, 256], F32, kind="ExternalInput").ap()
    smallc = dt("smallc", [128, 64], F32, kind="ExternalInput").ap()
    yT = dt("yT", [1024, S], F32, kind="ExternalOutput").ap()

    es = contextlib.ExitStack()
    with es:
        def sb(name, shape, dtype):
            return es.enter_context(nc.sbuf_tensor(name, shape, dtype))

        XT = [sb("XT%d" % i, [128, DC, TP], BF16) for i in range(2)]
        WU = [sb("WU%d" % i, [128, DC, 512], BF16) for i in range(2)]
        WA = sb("WA", [128, 8, 256], BF16)
        WX = sb("WX", [128, 8, 256], BF16)
        SMALL = sb("SMALL", [128, 64], F32)
        SMV = SMALL[:].rearrange("p (n j) -> p n j", j=8)
        LAB = {n: sb("lab_" + n, [128, 8], F32) for n in ("u", "lnv", "ser", "msk", "a8", "a16")}
        ONESF = sb("ONESF", [128, 8], F32)

        def pair(name, dtype=F32, w=TP):
            return [sb("%s%d" % (name, i), [128, w], dtype) for i in range(2)]

        T1, GATE, XC, R, IG, AA, A2, BT, H, Y = [pair(n) for n in
                                                ("T1", "GATE", "XC", "R", "IG", "AA", "A2", "BT", "H", "Y")]
        GXp = [pair("GXa"), pair("GXb")]
        XBUFp = [pair("XBUFa", F32, TP + 3), pair("XBUFb", F32, TP + 3)]
        XCB = pair("XCB", BF16)
        HS = sb("HS", [128, 2], F32)
        PS = [es.enter_context(nc.psum_tensor("ps%d" % i, [128, 512], F32)) for i in range(8)]
        es.enter_context(nc.Block())

        P = Prog(nc)
        V, A, PE, G = nc.vector, nc.scalar, nc.tensor, nc.gpsimd
        bXT = [Buf(), Buf()]
        bWU = [Buf(), Buf()]
        bC = Buf("consts")
        bLAB = Buf("lab")
        bk = lambda: [Buf(), Buf()]
        bT1, bGATE, bXC, bR, bIG, bAA, bA2, bBT, bH, bY, bXCB, bHS = [bk() for _ in range(12)]
        bGXp = [bk(), bk()]
        bXBUFp = [bk(), bk()]
        bPS = [Buf("ps%d" % i) for i in range(8)]
        PPROJ, PGA, PGX = (0, 1, 2, 3), (4, 5), (6, 7)

        P.dma("sync", "c", SMALL[:], smallc, writes=[bC])
        P.dma("gpsimd", "c", WA[:].rearrange("p (u k) j -> p u k j", k=2), wa.rearrange("u (k p) j -> p u k j", p=128), writes=[bC])
        P.dma("gpsimd", "c", WX[:].rearrange("p (u k) j -> p u k j", k=2), wx.rearrange("u (k p) j -> p u k j", p=128), writes=[bC])
        P.op("vector", lambda: V.memset(ONESF[:], 1.0), writes=[bC])

        lam = SMV[:, :, 7]
        lb = [bLAB]
        P.op("scalar", lambda: A.activation(out=LAB["u"][:], in_=lam, func=AF.Exp, scale=-1.0), reads=[bC], writes=lb)
        P.op("scalar", lambda: A.activation(out=LAB["lnv"][:], in_=LAB["u"][:], func=AF.Ln, bias=1.0, scale=1.0), reads=lb, writes=lb)
        P.op("vector", lambda: V.tensor_scalar(out=LAB["ser"][:], in0=LAB["u"][:], scalar1=-0.25, scalar2=1.0 / 3.0, op0=ALU.mult, op1=ALU.add),
             reads=lb, writes=lb)
        P.op("vector", lambda: V.tensor_tensor(out=LAB["ser"][:], in0=LAB["ser"][:], in1=LAB["u"][:], op=ALU.mult), reads=lb, writes=lb)
        P.op("vector", lambda: V.tensor_scalar(out=LAB["ser"][:], in0=LAB["ser"][:], scalar1=-1.0, scalar2=0.5, op0=ALU.mult, op1=ALU.add),
             reads=lb, writes=lb)
        P.op("vector", lambda: V.tensor_tensor(out=LAB["ser"][:], in0=LAB["ser"][:], in1=LAB["u"][:], op=ALU.mult), reads=lb, writes=lb)
        P.op("vector", lambda: V.tensor_scalar(out=LAB["ser"][:], in0=LAB["ser"][:], scalar1=-1.0, scalar2=1.0, op0=ALU.mult, op1=ALU.add),
             reads=lb, writes=lb)
        P.op("vector", lambda: V.tensor_tensor(out=LAB["ser"][:], in0=LAB["ser"][:], in1=LAB["u"][:], op=ALU.mult), reads=lb, writes=lb)
        P.op("vector", lambda: V.tensor_scalar(out=LAB["msk"][:], in0=LAB["u"][:], scalar1=0.05, scalar2=None, op0=ALU.is_lt), reads=lb, writes=lb)
        P.op("vector", lambda: V.tensor_tensor(out=LAB["ser"][:], in0=LAB["ser"][:], in1=LAB["lnv"][:], op=ALU.subtract), reads=lb, writes=lb)
        P.op("vector", lambda: V.tensor_tensor(out=LAB["ser"][:], in0=LAB["ser"][:], in1=LAB["msk"][:], op=ALU.mult), reads=lb, writes=lb)
        P.op("vector", lambda: V.tensor_tensor(out=LAB["ser"][:], in0=LAB["ser"][:], in1=LAB["lnv"][:], op=ALU.add), reads=lb, writes=lb)
        P.op("vector", lambda: V.tensor_scalar(out=LAB["a8"][:], in0=LAB["ser"][:], scalar1=-LRU_C, scalar2=None, op0=ALU.mult), reads=lb, writes=lb)
        P.op("vector", lambda: V.tensor_scalar(out=LAB["a16"][:], in0=LAB["ser"][:], scalar1=-2.0 * LRU_C, scalar2=None, op0=ALU.mult),
             reads=lb, writes=lb)

        xv = xT.rearrange("(c p) t -> p c t", p=128)

        def load_x(i, slot):
            for q in range(4):
                P.dma("gpsimd", "x", XT[slot][:, q * 8:(q + 1) * 8, :], xv[:, q * 8:(q + 1) * 8, i * TP:(i + 1) * TP],
                      writes=[bXT[slot]])

        def load_w(unit, slot):
            srcv = win[unit].rearrange("(c p) f -> p c f", p=128)
            for q in range(4):
                P.dma("gpsimd", "x", WU[slot][:, q * 8:(q + 1) * 8, :], srcv[:, q * 8:(q + 1) * 8, :], writes=[bWU[slot]])

        load_w(0, 0)
        load_x(0, 0)
        load_x(1, 1)
        items = [(u, i) for u in range(n_units) for i in range(NT)]

        def stage_a(n):
            unit, i = items[n]
            ws, xs_, par = unit % 2, n % 2, n % 2
            W, X = WU[ws], XT[xs_]
            for oc in range(4):
                pp = PPROJ[oc]
                for c in range(DC):
                    P.op("tensor", (lambda c=c, oc=oc, pp=pp, X=X, W=W: PE.matmul(PS[pp][:], lhsT=W[:, c, oc * 128:(oc + 1) * 128],
                                                                                   rhs=X[:, c, :], start=(c == 0), stop=(c == DC - 1))),
                         reads=[bWU[ws], bXT[xs_]], writes=[bPS[pp]])
                k = oc % 2
                if oc < 2:
                    P.op("scalar", (lambda k=k, pp=pp, par=par: A.copy(out=GXp[par][k][:], in_=PS[pp][:])),
                         reads=[bPS[pp]], writes=[bGXp[par][k]])
                else:
                    P.op("scalar", (lambda k=k, pp=pp, par=par: A.copy(out=XBUFp[par][k][:, 3:3 + TP], in_=PS[pp][:])),
                         reads=[bPS[pp]], writes=[bXBUFp[par][k]])
                    if i == 0:
                        P.op("vector", (lambda k=k, par=par: V.memset(XBUFp[par][k][:, 0:3], 0.0)),
                             reads=[bXBUFp[par][k]], writes=[bXBUFp[par][k]])
                    else:
                        P.op("vector", (lambda k=k, par=par: V.tensor_copy(out=XBUFp[par][k][:, 0:3], in_=XBUFp[1 - par][k][:, TP:TP + 3])),
                             reads=[bXBUFp[1 - par][k], bXBUFp[par][k]], writes=[bXBUFp[par][k]])
            nxt = n + 2
            if nxt < len(items):
                load_x(items[nxt][1], nxt % 2)
            if i == 0 and unit + 1 < n_units:
                load_w(unit + 1, (unit + 1) % 2)

        def stage_b(n):
            unit, i = items[n]
            par = n % 2
            for k in range(2):
                GXk = GXp[par][k]
                P.op("vector", (lambda k=k, GXk=GXk: V.tensor_tensor(out=T1[k][:], in0=GXk[:], in1=GXk[:], op=ALU.mult)),
                     reads=[bGXp[par][k], bT1[k]], writes=[bT1[k]])
                P.op("vector", (lambda k=k: V.tensor_scalar(out=T1[k][:], in0=T1[k][:], scalar1=2.0 * GELU_C * 0.044715, scalar2=2.0 * GELU_C,
                                                             op0=ALU.mult, op1=ALU.add)), reads=[bT1[k]], writes=[bT1[k]])
                P.op("vector", (lambda k=k, GXk=GXk: V.tensor_tensor(out=T1[k][:], in0=T1[k][:], in1=GXk[:], op=ALU.mult)),
                     reads=[bT1[k], bGXp[par][k]], writes=[bT1[k]])
                P.op("scalar", (lambda k=k: A.activation(out=T1[k][:], in_=T1[k][:], func=AF.Sigmoid)), reads=[bT1[k]], writes=[bT1[k]])
                P.op("gpsimd", (lambda k=k, GXk=GXk: G.tensor_tensor(out=GATE[k][:], in0=T1[k][:], in1=GXk[:], op=ALU.mult)),
                     reads=[bT1[k], bGXp[par][k], bGATE[k]], writes=[bGATE[k]])
            for k in range(2):
                nn = unit * 2 + k
                xb = XBUFp[par][k]
                P.op("vector", (lambda k=k, nn=nn, xb=xb: V.tensor_scalar(out=XC[k][:], in0=xb[:, 3:3 + TP], scalar1=SMV[:, nn, 3:4], scalar2=SMV[:, nn, 4:5],
                                                                           op0=ALU.mult, op1=ALU.add)),
                     reads=[bXBUFp[par][k], bC, bXC[k]], writes=[bXC[k]])
                for tap in (2, 1, 0):
                    P.op("vector", (lambda k=k, nn=nn, xb=xb, tap=tap: V.scalar_tensor_tensor(out=XC[k][:], in0=xb[:, tap:tap + TP], scalar=SMV[:, nn, tap:tap + 1],
                                                                                               in1=XC[k][:], op0=ALU.mult, op1=ALU.add)),
                         reads=[bXBUFp[par][k], bC, bXC[k]], writes=[bXC[k]])
                P.op("scalar", (lambda k=k: A.copy(out=XCB[k][:], in_=XC[k][:])), reads=[bXC[k]], writes=[bXCB[k]])

        def stage_c(n):
            unit, i = items[n]
            if i == 0:
                for k in range(2):
                    P.op("vector", (lambda k=k: V.memset(HS[:, k:k + 1], 0.0)), reads=[bHS[k]], writes=[bHS[k]])
            for j in range(2):
                nn = unit * 2 + j
                for (Wg, pg, dst, bdst, bcol) in ((WA, PGA[j], R, bR, 5), (WX, PGX[j], IG, bIG, 6)):
                    for k in range(2):
                        P.op("tensor", (lambda k=k, j=j, Wg=Wg, pg=pg, unit=unit: PE.matmul(PS[pg][:], lhsT=Wg[:, unit * 2 + k, j * 128:(j + 1) * 128],
                                                                                             rhs=XCB[k][:], start=(k == 0), stop=(k == 1))),
                             reads=[bC, bXCB[0], bXCB[1]], writes=[bPS[pg]])
                    P.op("scalar", (lambda j=j, nn=nn, pg=pg, dst=dst, bcol=bcol: A.activation(out=dst[j][:], in_=PS[pg][:], func=AF.Sigmoid,
                                                                                                bias=SMV[:, nn, bcol:bcol + 1], scale=1.0)),
                         reads=[bPS[pg], bC], writes=[bdst[j]])
            for j in range(2):
                nn = unit * 2 + j
                P.op("scalar", (lambda j=j, nn=nn: A.activation(out=AA[j][:], in_=R[j][:], func=AF.Exp, scale=LAB["a8"][:, nn:nn + 1])),
                     reads=[bR[j], bLAB], writes=[bAA[j]])
                P.op("scalar", (lambda j=j, nn=nn: A.activation(out=A2[j][:], in_=R[j][:], func=AF.Exp, scale=LAB["a16"][:, nn:nn + 1])),
                     reads=[bR[j], bLAB], writes=[bA2[j]])
            for j in range(2):
                P.op("vector", (lambda j=j: V.tensor_scalar(out=A2[j][:], in0=A2[j][:], scalar1=-1.0, scalar2=1.0, op0=ALU.mult, op1=ALU.add)),
                     reads=[bA2[j]], writes=[bA2[j]])
                P.op("vector", (lambda j=j: V.tensor_scalar(out=A2[j][:], in0=A2[j][:], scalar1=0.0, scalar2=None, op0=ALU.max)),
                     reads=[bA2[j]], writes=[bA2[j]])
            for j in range(2):
                P.op("scalar", (lambda j=j: A.activation(out=A2[j][:], in_=A2[j][:], func=AF.Sqrt)), reads=[bA2[j]], writes=[bA2[j]])
            for j in range(2):
                P.op("vector", (lambda j=j: V.tensor_tensor(out=BT[j][:], in0=A2[j][:], in1=IG[j][:], op=ALU.mult)),
                     reads=[bA2[j], bIG[j], bBT[j]], writes=[bBT[j]])
                P.op("vector", (lambda j=j: V.tensor_tensor(out=BT[j][:], in0=BT[j][:], in1=XC[j][:], op=ALU.mult)),
                     reads=[bBT[j], bXC[j]], writes=[bBT[j]])
                P.op("vector", (lambda j=j: V.tensor_tensor_scan(out=H[j][:], data0=AA[j][:], data1=BT[j][:], initial=HS[:, j:j + 1],
                                                                  op0=ALU.mult, op1=ALU.add)),
                     reads=[bAA[j], bBT[j], bHS[j], bH[j]], writes=[bH[j]])
                P.op("vector", (lambda j=j: V.tensor_copy(out=HS[:, j:j + 1], in_=H[j][:, TP - 1:TP])), reads=[bH[j]], writes=[bHS[j]])
                P.op("gpsimd", (lambda j=j: G.tensor_tensor(out=Y[j][:], in0=H[j][:], in1=GATE[j][:], op=ALU.mult)),
                     reads=[bH[j], bGATE[j], bY[j]], writes=[bY[j]])
                row = (unit * 2 + j) * 128
                P.dma("sync", "out", yT[row:row + 128, i * TP:(i + 1) * TP], Y[j][:], reads=[bY[j]])

        stage_a(0)
        for n in range(len(items)):
            stage_b(n)
            if n + 1 < len(items):
                stage_a(n + 1)
            stage_c(n)
        stats = P.emit(es)
    return nc, stats


_PROGS = {}


def _prog(name):
    if name not in _PROGS:
        if name == "tail":
            _PROGS[name] = build_tail(SEQ // 4, NE, True)[0]
        elif name == "mix0":
            _PROGS[name] = build_mix0(SEQ)[0]
        elif name == "mix1":
            _PROGS[name] = build_mix1(SEQ)[0]
    return _PROGS[name]


def _c(a):
    return np.ascontiguousarray(a, dtype=np.float32)


def _mix0_inputs(c, xT_b, w_in, w_pool, pscale, b_f, consts):
    g = c % 4
    heads = [4 * g + i for i in range(4)]
    wqkv = np.stack([np.concatenate([w_in[:, 2048 + h * 128:2048 + (h + 1) * 128],
                                     w_in[:, 4096 + h * 128:4096 + (h + 1) * 128],
                                     w_in[:, 6144 + h * 128:6144 + (h + 1) * 128]], 1) for h in heads])
    wf = w_in[:, 8192 + 4 * g:8192 + 4 * g + 4]
    wf_l = wf.reshape(DC, 128, 4).transpose(1, 0, 2).reshape(128, DC * 4)
    small = np.zeros((128, 64), np.float32)
    small[:, 0:4] = b_f[4 * g:4 * g + 4][None]
    small[:, 4:8] = pscale[g * 512:(g + 1) * 512].reshape(4, 128).T
    w = 2 ** (g + 1)
    small[:, 8 + g] = 1.0 / w
    small[:, 12:28] = (w / np.minimum(np.arange(16) + 1, w))[None]
    return dict(xT=xT_b, wp=_c(w_in[:, g * 512:(g + 1) * 512]), wqkv=_c(wqkv), wf=_c(wf_l), smallc=small,
                wpool=_c(w_pool[g]), **consts)


def _mix1_inputs(c, xT_b, w_in, conv_w, conv_b, w_a, b_a, w_x, b_x, lam):
    qt = c % 4
    ch0 = qt * 1024
    win = np.stack([np.concatenate([w_in[:, ch0 + u * 256:ch0 + (u + 1) * 256],
                                    w_in[:, D + ch0 + u * 256:D + ch0 + (u + 1) * 256]], 1) for u in range(4)])
    small = np.zeros((128, 8, 8), np.float32)
    b_a = b_a.reshape(-1)
    b_x = b_x.reshape(-1)
    for n in range(8):
        sl = slice(ch0 + n * 128, ch0 + (n + 1) * 128)
        for i in range(4):
            small[:, n, i] = conv_w[i, sl]
        small[:, n, 4] = conv_b[sl]
        small[:, n, 5] = b_a[sl]
        small[:, n, 6] = b_x[sl]
        small[:, n, 7] = lam[sl]
    return dict(xT=xT_b, win=_c(win), wa=_c(w_a[qt * 4:qt * 4 + 4]), wx=_c(w_x[qt * 4:qt * 4 + 4]),
                smallc=small.reshape(128, 64))


def _tail_inputs(layer, xresT, mixT, wout, moe_w_group, moe_b_group, moe_w_expert, moe_b_expert,
                 moe_w1, moe_w3, moe_w2, ln_g, ln_b, consts):
    wr = _c(np.concatenate([moe_w_group[layer], moe_w_expert[layer]], 1))
    br = _c(np.tile(np.concatenate([moe_b_group[layer], moe_b_expert[layer]])[None], (128, 1)))
    lnp = _c(np.stack([pc(ln_g[layer, 0]), pc(ln_b[layer, 0]), pc(ln_g[layer, 1]), pc(ln_b[layer, 1])], 1))
    common = dict(wr=wr, br=br, w1=_c(moe_w1[layer]), w3=_c(moe_w3[layer]), w2=_c(moe_w2[layer]),
                  lnp=lnp, wout=_c(wout), **consts)
    return [dict(xres=xresT[c], mix=mixT[c], **common) for c in range(8)]


def kernel(x, even_w_in, even_w_pool, even_pool_scale, even_b_f, even_w_out,
           odd_w_in, odd_conv_w, odd_conv_b, odd_w_a, odd_b_a, odd_w_x, odd_b_x,
           odd_lambda, odd_w_out, moe_w_group, moe_b_group, moe_w_expert, moe_b_expert,
           moe_w1, moe_w3, moe_w2, ln_g, ln_b):
    f = lambda a: np.asarray(a, dtype=np.float32)
    x = f(x)
    cores = list(range(8))
    TQ = SEQ // 4
    xT = [_c(x[b].T) for b in range(BATCH)]

    c0 = mix0_consts()
    in0 = [_mix0_inputs(c, xT[c // 4], f(even_w_in[0]), f(even_w_pool[0]), f(even_pool_scale[0]), f(even_b_f[0]), c0)
           for c in cores]
    r0 = run_bass_kernel_spmd(_prog("mix0"), in0, core_ids=cores).results
    mixT = []
    for c in cores:
        b, j = c // 4, c % 4
        ts = slice(j * TQ, (j + 1) * TQ)
        mixT.append(_c(np.concatenate([r0[4 * b + g]["aT"][:, ts] for g in range(4)] +
                                      [r0[4 * b + g]["bT"][:, ts] for g in range(4)], 0)))
    xres = [_c(xT[c // 4][:, (c % 4) * TQ:(c % 4 + 1) * TQ]) for c in cores]
    tc = tail_consts()
    moe = (f(moe_w_group), f(moe_b_group), f(moe_w_expert), f(moe_b_expert), moe_w1, moe_w3, moe_w2, f(ln_g), f(ln_b))
    r1 = run_bass_kernel_spmd(_prog("tail"), _tail_inputs(0, xres, mixT, f(even_w_out[0]), *moe, tc), core_ids=cores).results
    x2 = [r1[c]["xout"] for c in cores]

    x2T = [_c(np.concatenate([x2[4 * b + j] for j in range(4)], 1)) for b in range(BATCH)]
    in2 = [_mix1_inputs(c, x2T[c // 4], f(odd_w_in[0]), f(odd_conv_w[0]), f(odd_conv_b[0]), f(odd_w_a[0]), f(odd_b_a[0]),
                        f(odd_w_x[0]), f(odd_b_x[0]), f(odd_lambda[0])) for c in cores]
    r2 = run_bass_kernel_spmd(_prog("mix1"), in2, core_ids=cores).results
    mixT = []
    for c in cores:
        b, j = c // 4, c % 4
        ts = slice(j * TQ, (j + 1) * TQ)
        mixT.append(_c(np.concatenate([r2[4 * b + q]["yT"][:, ts] for q in range(4)], 0)))
    r3 = run_bass_kernel_spmd(_prog("tail"), _tail_inputs(1, x2, mixT, f(odd_w_out[0]), *moe, tc), core_ids=cores).results
    out = np.empty((BATCH, SEQ, D), np.float32)
    for c in cores:
        b, j = c // 4, c % 4
        out[b, j * TQ:(j + 1) * TQ, :] = r3[c]["xout"].T
    return out
```
